# Optimizing a Trainium2 kernel written in Bass

```python
import jax, jax.numpy as jnp
from jax import lax
import numpy as np


D_MODEL = 2048
BATCH = 2
SEQ = 4096
DEPTH = 1

HEAD_DIM = 128
MIX_WIDTH = D_MODEL
NSA_HEADS = MIX_WIDTH // 2 // HEAD_DIM
NSA_KV_GROUPS = 2
NSA_HPG = NSA_HEADS // NSA_KV_GROUPS
MOBA_HEADS = MIX_WIDTH // 2 // HEAD_DIM
CMP_LEN = 32
CMP_STRIDE = 16
CMP_HIDDEN = HEAD_DIM
SEL_LEN = 64
SEL_TOPK = 16
WINDOW = 512
MOBA_BLOCK = 256
MOBA_TOPK = 3
D_FF = 4 * D_MODEL
ROPE_THETA = 10000.0
EPS = 1e-6
NSA_Q_CHUNK = 64
MOBA_Q_CHUNK = 16

NSA_Q_W = NSA_HEADS * HEAD_DIM
NSA_KV_W = NSA_KV_GROUPS * HEAD_DIM
NSA_GATE_W = 3 * NSA_HEADS
MOBA_W = MOBA_HEADS * HEAD_DIM
IN_WIDTHS = (NSA_Q_W,) + (NSA_KV_W,) * 6 + (NSA_GATE_W, MOBA_W, MOBA_W, MOBA_W)
IN_COLS = sum(IN_WIDTHS)

kernel_name = 'hymba_nsa_moba_sandwich_adaln_block'


def rms_norm(x, g):
    x32 = x.astype(jnp.float32)
    y = x32 * lax.rsqrt(jnp.mean(x32 * x32, axis=-1, keepdims=True) + EPS)
    return (y * g.astype(jnp.float32)).astype(x.dtype)


def modulate(h, shift, scale):
    return h * (1.0 + scale[:, None, :]) + shift[:, None, :]


def rope_tables(T):
    pos = jnp.arange(T, dtype=jnp.float32)
    inv = ROPE_THETA ** (-jnp.arange(0, HEAD_DIM, 2, dtype=jnp.float32) / HEAD_DIM)
    ang = pos[:, None] * inv[None, :]
    return jnp.cos(ang), jnp.sin(ang)


def apply_rope(x, cos, sin):
    shp = (x.shape[1],) + (1,) * (x.ndim - 3) + (HEAD_DIM // 2,)
    c_ = cos.reshape(shp)
    s_ = sin.reshape(shp)
    x32 = x.astype(jnp.float32)
    x1, x2 = jnp.split(x32, 2, axis=-1)
    return jnp.concatenate([x1 * c_ - x2 * s_, x2 * c_ + x1 * s_], axis=-1).astype(x.dtype)


def masked_softmax(s, mask):
    s = jnp.where(mask, s.astype(jnp.float32), -jnp.inf)
    m = jnp.max(s, axis=-1, keepdims=True)
    m = jnp.where(jnp.isfinite(m), m, 0.0)
    e = jnp.exp(s - m)
    return e / jnp.maximum(jnp.sum(e, axis=-1, keepdims=True), jnp.float32(1e-30))


def compress(x, pos, w1, w2):
    B, T, G, dh = x.shape
    n_cmp = (T - CMP_LEN) // CMP_STRIDE + 1
    idx = np.arange(n_cmp)[:, None] * CMP_STRIDE + np.arange(CMP_LEN)[None, :]
    blocks = x[:, idx] + pos[:, None, :].astype(x.dtype)
    blocks = blocks.transpose(0, 1, 3, 2, 4).reshape(B, n_cmp, G, CMP_LEN * dh)
    return jax.nn.silu(blocks @ w1) @ w2


def nsa_mixer(q, k_cmp, v_cmp, k_sel, v_sel, k_win, v_win, gate_logits,
              cmp_k_pos, cmp_k_w1, cmp_k_w2, cmp_v_pos, cmp_v_w1, cmp_v_w2, cos, sin):
    B, T, _ = q.shape
    G, Hg, dh = NSA_KV_GROUPS, NSA_HPG, HEAD_DIM
    Qc = NSA_Q_CHUNK
    q = apply_rope(q.reshape(B, T, G, Hg, dh), cos, sin) * (dh ** -0.5)
    kv = lambda a: a.reshape(B, T, G, dh)
    k_cmp = apply_rope(kv(k_cmp), cos, sin)
    k_sel = apply_rope(kv(k_sel), cos, sin)
    k_win = apply_rope(kv(k_win), cos, sin)
    v_cmp, v_sel, v_win = kv(v_cmp), kv(v_sel), kv(v_win)
    gates = jax.nn.sigmoid(gate_logits.astype(jnp.float32)).reshape(B, T, G, Hg, 3)

    kc = compress(k_cmp, cmp_k_pos, cmp_k_w1, cmp_k_w2)
    vc = compress(v_cmp, cmp_v_pos, cmp_v_w1, cmp_v_w2)
    n_cmp = kc.shape[1]
    cmp_start = np.arange(n_cmp) * CMP_STRIDE
    cmp_end_np = cmp_start + CMP_LEN
    cmp_end = jnp.asarray(cmp_end_np - 1)

    n_sel = T // SEL_LEN
    k_top = min(SEL_TOPK, n_sel)
    sel_start = np.arange(n_sel) * SEL_LEN
    overlap = np.clip(np.minimum(cmp_end_np[:, None], sel_start[None, :] + SEL_LEN)
                      - np.maximum(cmp_start[:, None], sel_start[None, :]), 0, None)
    sel_map = jnp.asarray((overlap / CMP_LEN).astype(np.float32))
    ks_blk = k_sel.reshape(B, n_sel, SEL_LEN, G, dh).transpose(0, 3, 1, 2, 4)
    vs_blk = v_sel.reshape(B, n_sel, SEL_LEN, G, dh).transpose(0, 3, 1, 2, 4)
    bi = jnp.arange(B)[:, None, None, None]
    gi = jnp.arange(G)[None, :, None, None]

    pad = ((0, 0), (WINDOW, 0), (0, 0), (0, 0))
    kw_pad = jnp.pad(k_win, pad)
    vw_pad = jnp.pad(v_win, pad)

    def chunk(i):
        s0 = i * Qc
        qc = lax.dynamic_slice_in_dim(q, s0, Qc, axis=1)
        gc = lax.dynamic_slice_in_dim(gates, s0, Qc, axis=1)
        tq = s0 + jnp.arange(Qc)
        sc = jnp.einsum('bqghd,bcgd->bghqc', qc, kc)
        p_c = masked_softmax(sc, cmp_end[None, :] <= tq[:, None])
        o_c = jnp.einsum('bghqc,bcgd->bqghd', p_c.astype(vc.dtype), vc)
        imp = jnp.einsum('bghqc,cs->bgqs', p_c, sel_map)
        cb = tq // SEL_LEN
        blk = jnp.arange(n_sel)[None, :]
        causal = blk <= cb[:, None]
        forced = (blk == 0) | (blk == cb[:, None]) | (blk == cb[:, None] - 1)
        score = jnp.where(causal, jnp.where(forced, jnp.inf, imp), -jnp.inf)
        _, idx = lax.top_k(score, k_top)
        ksg = ks_blk[bi, gi, idx]
        vsg = vs_blk[bi, gi, idx]
        kpos = idx[..., None] * SEL_LEN + jnp.arange(SEL_LEN)
        mask_s = (kpos <= tq[:, None, None]).reshape(B, G, 1, Qc, k_top * SEL_LEN)
        ss = jnp.einsum('bqghd,bgqkld->bghqkl', qc, ksg).reshape(B, G, Hg, Qc, k_top * SEL_LEN)
        p_s = masked_softmax(ss, mask_s)
        o_s = jnp.einsum('bghqn,bgqnd->bqghd', p_s.astype(vsg.dtype),
                         vsg.reshape(B, G, Qc, k_top * SEL_LEN, dh))
        kw = lax.dynamic_slice_in_dim(kw_pad, s0, Qc + WINDOW, axis=1)
        vw = lax.dynamic_slice_in_dim(vw_pad, s0, Qc + WINDOW, axis=1)
        kp = s0 - WINDOW + jnp.arange(Qc + WINDOW)
        dist = tq[:, None] - kp[None, :]
        mask_w = (dist >= 0) & (dist < WINDOW) & (kp[None, :] >= 0)
        sw = jnp.einsum('bqghd,bkgd->bghqk', qc, kw)
        p_w = masked_softmax(sw, mask_w)
        o_w = jnp.einsum('bghqk,bkgd->bqghd', p_w.astype(vw.dtype), vw)
        g = gc.astype(o_c.dtype)
        o = g[..., 0:1] * o_c + g[..., 1:2] * o_s + g[..., 2:3] * o_w
        return o.reshape(B, Qc, NSA_HEADS * dh)

    out = lax.map(chunk, jnp.arange(T // Qc))
    return jnp.moveaxis(out, 0, 1).reshape(B, T, NSA_HEADS * dh)


def moba_mixer(q, k, v, cos, sin):
    B, T, _ = q.shape
    H, dh, BLK, Qc = MOBA_HEADS, HEAD_DIM, MOBA_BLOCK, MOBA_Q_CHUNK
    q = apply_rope(q.reshape(B, T, H, dh), cos, sin) * (dh ** -0.5)
    k = apply_rope(k.reshape(B, T, H, dh), cos, sin)
    v = v.reshape(B, T, H, dh)
    nb = -(-T // BLK)
    pad = ((0, 0), (0, nb * BLK - T), (0, 0), (0, 0))
    k_pad = jnp.pad(k, pad)
    v_pad = jnp.pad(v, pad)
    k_blk = k_pad.reshape(B, nb, BLK, H, dh).transpose(0, 3, 1, 2, 4)
    v_blk = v_pad.reshape(B, nb, BLK, H, dh).transpose(0, 3, 1, 2, 4)
    k_mean = jnp.mean(k_blk.astype(jnp.float32), axis=3).astype(k.dtype)
    k_past = min(MOBA_TOPK, nb - 1)
    bi = jnp.arange(B)[:, None, None, None]
    hi = jnp.arange(H)[None, :, None, None]

    def chunk(i):
        s0 = i * Qc
        qc = lax.dynamic_slice_in_dim(q, s0, Qc, axis=1)
        tq = s0 + jnp.arange(Qc)
        cur = s0 // BLK
        ko = lax.dynamic_slice_in_dim(k_pad, cur * BLK, BLK, axis=1)
        vo = lax.dynamic_slice_in_dim(v_pad, cur * BLK, BLK, axis=1)
        opos = cur * BLK + jnp.arange(BLK)
        s_own = jnp.einsum('bqhd,blhd->bhql', qc, ko)
        m_own = jnp.broadcast_to(opos[None, :] <= tq[:, None], (B, H, Qc, BLK))
        if k_past > 0:
            gsc = jnp.einsum('bqhd,bhnd->bhqn', qc, k_mean).astype(jnp.float32)
            gsc = jnp.where(jnp.arange(nb) < cur, gsc, -jnp.inf)
            _, idx = lax.top_k(gsc, k_past)
            kg = k_blk[bi, hi, idx]
            vg = v_blk[bi, hi, idx]
            s_past = jnp.einsum('bqhd,bhqkld->bhqkl', qc, kg).reshape(B, H, Qc, k_past * BLK)
            m_past = jnp.broadcast_to((idx < cur)[..., None],
                                      (B, H, Qc, k_past, BLK)).reshape(B, H, Qc, k_past * BLK)
            p = masked_softmax(jnp.concatenate([s_past, s_own], axis=-1),
                               jnp.concatenate([m_past, m_own], axis=-1))
            p = p.astype(v.dtype)
            o = (jnp.einsum('bhqn,bhqnd->bqhd', p[..., :k_past * BLK],
                            vg.reshape(B, H, Qc, k_past * BLK, dh))
                 + jnp.einsum('bhql,blhd->bqhd', p[..., k_past * BLK:], vo))
        else:
            p = masked_softmax(s_own, m_own).astype(v.dtype)
            o = jnp.einsum('bhql,blhd->bqhd', p, vo)
        return o.reshape(B, Qc, H * dh)

    out = lax.map(chunk, jnp.arange(T // Qc))
    return jnp.moveaxis(out, 0, 1).reshape(B, T, H * dh)


def setup_inputs(seed: int = 0) -> dict:
    key = jax.random.key(seed)
    ks = jax.random.split(key, 18)
    nrm = lambda k, shape, s: jax.random.normal(k, shape, jnp.float32) * s
    L = DEPTH
    return {
        'x': nrm(ks[0], (BATCH, SEQ, D_MODEL), 1.0),
        'c': nrm(ks[1], (BATCH, D_MODEL), 1.0),
        'w_ada': nrm(ks[2], (L, D_MODEL, 6 * D_MODEL), 0.5 * D_MODEL ** -0.5),
        'b_ada': nrm(ks[3], (L, 6 * D_MODEL), 0.01),
        'pre_norm_mix': 1.0 + nrm(ks[4], (L, D_MODEL), 0.05),
        'post_norm_mix': 1.0 + nrm(ks[5], (L, D_MODEL), 0.05),
        'w_in': nrm(ks[6], (L, D_MODEL, IN_COLS), D_MODEL ** -0.5),
        'cmp_k_pos': nrm(ks[7], (L, CMP_LEN, HEAD_DIM), 0.1),
        'cmp_k_w1': nrm(ks[8], (L, CMP_LEN * HEAD_DIM, CMP_HIDDEN), (CMP_LEN * HEAD_DIM) ** -0.5),
        'cmp_k_w2': nrm(ks[9], (L, CMP_HIDDEN, HEAD_DIM), CMP_HIDDEN ** -0.5),
        'cmp_v_pos': nrm(ks[10], (L, CMP_LEN, HEAD_DIM), 0.1),
        'cmp_v_w1': nrm(ks[11], (L, CMP_LEN * HEAD_DIM, CMP_HIDDEN), (CMP_LEN * HEAD_DIM) ** -0.5),
        'cmp_v_w2': nrm(ks[12], (L, CMP_HIDDEN, HEAD_DIM), CMP_HIDDEN ** -0.5),
        'w_o': nrm(ks[13], (L, MIX_WIDTH, D_MODEL), MIX_WIDTH ** -0.5),
        'pre_norm_ffn': 1.0 + nrm(ks[14], (L, D_MODEL), 0.05),
        'post_norm_ffn': 1.0 + nrm(ks[15], (L, D_MODEL), 0.05),
        'w_up': nrm(ks[16], (L, D_MODEL, D_FF), D_MODEL ** -0.5),
        'w_down': nrm(ks[17], (L, D_FF, D_MODEL), D_FF ** -0.5),
    }


def reference(x, c, w_ada, b_ada, pre_norm_mix, post_norm_mix, w_in,
              cmp_k_pos, cmp_k_w1, cmp_k_w2, cmp_v_pos, cmp_v_w1, cmp_v_w2,
              w_o, pre_norm_ffn, post_norm_ffn, w_up, w_down):
    T = x.shape[1]
    cos, sin = rope_tables(T)
    split_at = np.cumsum(IN_WIDTHS)[:-1].tolist()
    for l in range(DEPTH):
        mod = jax.nn.silu(c) @ w_ada[l] + b_ada[l]
        shift_m, scale_m, gate_m, shift_f, scale_f, gate_f = jnp.split(mod, 6, axis=-1)
        h = modulate(rms_norm(x, pre_norm_mix[l]), shift_m, scale_m)
        (q_n, k_c, v_c, k_s, v_s, k_w, v_w, g_n,
         q_m, k_m, v_m) = jnp.split(h @ w_in[l], split_at, axis=-1)
        o_n = nsa_mixer(q_n, k_c, v_c, k_s, v_s, k_w, v_w, g_n,
                        cmp_k_pos[l], cmp_k_w1[l], cmp_k_w2[l],
                        cmp_v_pos[l], cmp_v_w1[l], cmp_v_w2[l], cos, sin)
        o_m = moba_mixer(q_m, k_m, v_m, cos, sin)
        o = jnp.concatenate([o_n, o_m], axis=-1) @ w_o[l]
        x = x + gate_m[:, None, :] * rms_norm(o, post_norm_mix[l])
        h = modulate(rms_norm(x, pre_norm_ffn[l]), shift_f, scale_f)
        f = jnp.square(jax.nn.relu(h @ w_up[l])) @ w_down[l]
        x = x + gate_f[:, None, :] * rms_norm(f, post_norm_ffn[l])
    return x
```

```python
import os
import contextlib
import numpy as np
import ml_dtypes
import concourse.bass as bass
import concourse.mybir as mybir
from concourse.bass_utils import run_bass_kernel_spmd

F32 = mybir.dt.float32
BF16 = mybir.dt.bfloat16
AF = mybir.ActivationFunctionType
ALU = mybir.AluOpType
AX = mybir.AxisListType

D = 2048
T = 4096
NT = 32
NL = 8
KC = 16
DFF = 8192
EPS = 1e-6
NEG = 8192.0
ARENA_BYTES = 176 * 1024

C_QN, C_KC, C_VC, C_KS, C_VS, C_KW, C_VW, C_G, C_QM, C_KM, C_VM = (
    0, 1024, 1280, 1536, 1792, 2048, 2304, 2560, 2584, 3608, 4632)
FM = [(C_KC, True), (C_KC + 128, True), (C_VC, False), (C_VC + 128, False),
      (C_KS, True), (C_KS + 128, True), (C_KW, True), (C_KW + 128, True)] + \
     [(C_KM + 128 * h, True) for h in range(8)]
TM = [C_VS, C_VS + 128, C_VW, C_VW + 128] + [C_VM + 128 * h for h in range(8)]


class Buf:
    __slots__ = ("w", "r")

    def __init__(self):
        self.w = None
        self.r = {}


class Prog:
    ENGS = ("pe", "act", "dve", "pool", "sp")

    def __init__(self, nc):
        self.nc = nc
        self.ops = {e: [] for e in self.ENGS}
        self.cnt = {e: 0 for e in self.ENGS}
        self.dcnt = {}

    def _deps(self, eng, reads, writes):
        waits = {}

        def need(s, v):
            if eng == "pe" and s == "pe":
                return
            if waits.get(s, 0) < v:
                waits[s] = v
        for b in reads:
            if b.w is not None:
                need(*b.w)
        for b in writes:
            if b.w is not None:
                need(*b.w)
            for s, v in b.r.items():
                need(s, v)
        return waits

    def _commit(self, tok, reads, writes):
        s, v = tok
        for b in reads:
            if b.r.get(s, 0) < v:
                b.r[s] = v
        for b in writes:
            b.w = tok
            b.r = {}

    def op(self, eng, fn, reads=(), writes=()):
        waits = self._deps(eng, reads, writes)
        self.cnt[eng] += 1
        tok = (eng, self.cnt[eng])
        self._commit(tok, reads, writes)
        self.ops[eng].append((waits, fn, eng, 1))

    def dma(self, q, sem, fn, reads=(), writes=()):
        waits = self._deps(q, reads, writes)
        self.dcnt[sem] = self.dcnt.get(sem, 0) + 16
        tok = (sem, self.dcnt[sem])
        self._commit(tok, reads, writes)
        self.ops[q].append((waits, fn, sem, 16))

    def barrier(self):
        waits = {e: c for e, c in self.cnt.items() if c > 0 and e != "sp"}
        waits.update(self.dcnt)
        for e in self.ENGS:
            w = dict(waits)
            if e == "pe":
                w.pop("pe", None)
            self.ops[e].append((w, None, None, 0))

    def final_wait(self, eng, bufs):
        waits = self._deps(eng, bufs, bufs)
        self.ops[eng].append((waits, None, None, 0))

    def emit(self):
        nc = self.nc
        semnames = list(self.ENGS[:4]) + sorted(self.dcnt.keys())
        with contextlib.ExitStack() as st:
            sems = {n: st.enter_context(nc.semaphore("s_" + n)) for n in semnames}
            block = st.enter_context(nc.Block())
            engmap = {"pe": block.tensor, "act": block.scalar, "dve": block.vector,
                      "pool": block.gpsimd, "sp": block.sync}
            for e in self.ENGS:
                ops = self.ops[e]
                if not ops:
                    continue

                def section(engine, ops=ops):
                    known = {}
                    for waits, fn, semn, inc in ops:
                        for s, v in waits.items():
                            if known.get(s, 0) < v:
                                engine.wait_ge(sems[s], v)
                                known[s] = v
                        if fn is not None:
                            fn(engine).then_inc(sems[semn], inc)
                engmap[e](section)


class TV:
    __slots__ = ("ap", "b")

    def __init__(self, ap):
        self.ap = ap
        self.b = Buf()


class Arena:
    def __init__(self, tens, nbytes):
        self.T = tens
        self.cap = nbytes
        self.top = 0

    def alloc(self, shape, dtype):
        n = int(np.prod(shape[1:]))
        esz = 4 if dtype == F32 else 2
        nb = (n * esz + 63) // 64 * 64
        off = self.top
        self.top += nb
        assert self.top <= self.cap, ("SBUF arena overflow", self.top, self.cap)
        ap = self.T[:, off // 4:(off + nb) // 4]
        if dtype == BF16:
            ap = ap.bitcast(BF16)
        ap = ap[:, 0:n]
        if len(shape) == 3:
            ap = ap.rearrange("p (a b) -> p a b", a=shape[1])
        elif len(shape) == 4:
            ap = ap.rearrange("p (a b c) -> p a b c", a=shape[1], b=shape[2])
        return TV(ap)


def build_program(debug=False, stop_after=None, skip=()):
    nc = bass.Bass("TRN2", target_bir_lowering=False)

    def din(name, shape, dt=F32):
        return nc.dram_tensor(name, list(shape), dt, kind="ExternalInput").ap()

    def dscr(name, shape, dt):
        return nc.dram_tensor(name, list(shape), dt, kind="ExternalOutput" if debug else "Internal").ap()

    xb = din("xb", [T, D])
    xo = din("xo", [NL * 128, D])
    wadaT = din("wadaT", [6 * D, D])
    bada = din("bada", [128, 96])
    cbc = din("cbc", [128, D])
    normsT = din("normsT", [128, 64])
    w_in_r = din("w_in_r", [22, 128, KC * 256])
    w_g_r = din("w_g_r", [128, KC * 24])
    cw1 = [din("ckw1", [128, 32 * 128]), din("cvw1", [128, 32 * 128])]
    cw2 = [din("ckw2", [128, 128]), din("cvw2", [128, 128])]
    cposT = [din("ckposT", [128, 32]), din("cvposT", [128, 32])]
    w_o_r = din("w_o_r", [4, 128, KC * 512])
    w_up_r = din("w_up_r", [64, 128, KC * 128])
    w_down_r = din("w_down_r", [32, 128, 8 * 512])
    cosk = din("cosk", [128, T])
    sink = din("sink", [128, T])
    cosq = din("cosq", [128, NL * 128])
    sinq = din("sinq", [128, NL * 128])
    cmask_d = din("cmask", [128, 4 * 128], BF16)
    wmask_d = din("wmask", [128, 8 * 128], BF16)
    cmpf_d = din("cmpf", [128, NL * 256])
    cmpb_d = din("cmpb", [128, NL * 256], BF16)
    nsab_d = din("nsab", [128, NL * 64])
    mobab_d = din("mobab", [128, NL * 16])
    mobav_d = din("mobav", [128, NL * 16])
    idb4_d = din("idb4", [128, 512], BF16)
    idf_d = din("idf", [128, 128])
    out = nc.dram_tensor("out", [NL * 128, D], F32, kind="ExternalOutput").ap()
    KT = dscr("KT", [16, 128, T], BF16)
    VD = dscr("VD", [NT, 128, 12 * 129], BF16)
    X1S = dscr("X1S", [NL * 128, D], F32)
    FS = dscr("FSC", [NL * 128, D], F32)
    if debug:
        dbg_mod = dscr("dbg_mod", [128, 96], F32)
        dbg_qt = dscr("dbg_qt", [128, 16 * 1024], BF16)
        dbg_g = dscr("dbg_g", [128, NL * 24], F32)
        dbg_ot = dscr("dbg_ot", [128, 16 * 1024], BF16)
        dbg_kc = dscr("dbg_kc", [128, 2 * 256], BF16)
        dbg_vc = dscr("dbg_vc", [128, 4 * 129], BF16)
        dbg_km = dscr("dbg_km", [128, 8 * 16], BF16)

    kt_b = {}
    vd_b = {}
    x1_b = [Buf() for _ in range(NL)]
    fs_b = {}
    out_b = [Buf() for _ in range(NL)]
    dbg_b = [Buf() for _ in range(8)]

    def ktb(idx, tc):
        return kt_b.setdefault((idx, tc), Buf())

    def vdb(gt, grp):
        return vd_b.setdefault((gt, grp), Buf())

    def fsb(i, cg):
        return fs_b.setdefault((i, cg), Buf())

    with contextlib.ExitStack() as st:
        arena_t = st.enter_context(nc.sbuf_tensor("arena", [128, ARENA_BYTES // 4], F32))
        PS = [TV(st.enter_context(nc.psum_tensor("ps%d" % k, [128, 512], F32))[:]) for k in range(8)]
        ar = Arena(arena_t, ARENA_BYTES)
        p = Prog(nc)
        op = p.op

        semmap = {}

        def dma(q, sem, out_ap, in_ap, reads=(), writes=()):
            if sem not in semmap:
                semmap[sem] = "d%02d" % len(semmap)
            p.dma(q, semmap[sem], lambda e: e.dma_start(out=out_ap, in_=in_ap), reads=reads, writes=writes)

        _raw_barrier = p.barrier

        def _barrier():
            _raw_barrier()
            semmap.clear()
        p.barrier = _barrier

        IDF = ar.alloc([128, 128], F32)
        IDB4 = ar.alloc([128, 512], BF16)
        MODT = ar.alloc([128, 96], F32)
        NRM = ar.alloc([128, 4, 16], F32)
        AM = ar.alloc([128, 16], F32)
        AFF = ar.alloc([128, 16], F32)
        AM8 = ar.alloc([128, 16], F32)
        AFF8 = ar.alloc([128, 16], F32)
        GGS = ar.alloc([128, 2, 16], F32)
        SSF = ar.alloc([128, NL, 4], F32)
        NST = [ar.alloc([128, 4], F32) for _ in range(2)]
        dma("sp", "c0", IDF.ap, idf_d, writes=[IDF.b])
        IDB1 = ar.alloc([128, 128], BF16)
        op("dve", lambda e: e.tensor_copy(out=IDB1.ap, in_=IDF.ap), reads=[IDF.b], writes=[IDB1.b])
        dma("sp", "c1", IDB4.ap, idb4_d, writes=[IDB4.b])
        dma("sp", "c2", NRM.ap, normsT.rearrange("p (a b) -> p a b", a=4), writes=[NRM.b])
        persist_mark = ar.top
        SLOT1 = persist_mark
        SLOT2 = persist_mark + 32 * 1024
        PB = persist_mark + 64 * 1024
        ar.top = PB
        G = ar.alloc([128, NL, 24], F32)
        KCT = ar.alloc([128, 2, 256], BF16)
        VCP = ar.alloc([128, 2, 2, 129], BF16)
        KMT = ar.alloc([128, 8, 16], BF16)
        ZR = ar.alloc([128, 258], BF16)
        SLOT3 = ar.top
        ar.top = persist_mark

        def phase_mod():
            SCB = ar.alloc([128, D], F32)
            BAD = ar.alloc([128, 96], F32)
            JNK = ar.alloc([128, D], BF16)
            WT = [ar.alloc([128, D], F32) for _ in range(3)]
            dma("sp", "m0", SCB.ap, cbc, writes=[SCB.b])
            dma("sp", "m1", BAD.ap, bada, writes=[BAD.b])
            op("act", lambda e: e.activation(out=SCB.ap, in_=SCB.ap, func=AF.Silu), reads=[SCB.b], writes=[SCB.b])
            for m in range(96):
                w = WT[m % 3]
                dma("sp", "mw%d" % (m % 3), w.ap, wadaT[m * 128:(m + 1) * 128, :], writes=[w.b])
                op("dve", lambda e, w=w, m=m: e.scalar_tensor_tensor(
                    out=JNK.ap, in0=w.ap, scalar=1.0, in1=SCB.ap, op0=ALU.mult, op1=ALU.mult,
                    accum_out=MODT.ap[:, m:m + 1]), reads=[w.b, SCB.b], writes=[JNK.b, MODT.b])
            op("dve", lambda e: e.tensor_tensor(out=MODT.ap, in0=MODT.ap, in1=BAD.ap, op=ALU.add),
               reads=[MODT.b, BAD.b], writes=[MODT.b])
            op("dve", lambda e: e.scalar_tensor_tensor(out=AM.ap, in0=MODT.ap[:, 16:32], scalar=1.0, in1=NRM.ap[:, 0, :],
                                                        op0=ALU.add, op1=ALU.mult), reads=[MODT.b, NRM.b], writes=[AM.b])
            op("dve", lambda e: e.scalar_tensor_tensor(out=AFF.ap, in0=MODT.ap[:, 64:80], scalar=1.0, in1=NRM.ap[:, 2, :],
                                                        op0=ALU.add, op1=ALU.mult), reads=[MODT.b, NRM.b], writes=[AFF.b])
            op("dve", lambda e: e.tensor_scalar(out=AM8.ap, in0=AM.ap, scalar1=1.0 / NEG, scalar2=None, op0=ALU.mult), reads=[AM.b], writes=[AM8.b])
            op("dve", lambda e: e.tensor_scalar(out=AFF8.ap, in0=AFF.ap, scalar1=1.0 / NEG, scalar2=None, op0=ALU.mult), reads=[AFF.b], writes=[AFF8.b])
            op("dve", lambda e: e.tensor_tensor(out=GGS.ap[:, 0, :], in0=MODT.ap[:, 32:48], in1=NRM.ap[:, 1, :], op=ALU.mult),
               reads=[MODT.b, NRM.b], writes=[GGS.b])
            op("dve", lambda e: e.tensor_tensor(out=GGS.ap[:, 1, :], in0=MODT.ap[:, 80:96], in1=NRM.ap[:, 3, :], op=ALU.mult),
               reads=[MODT.b, NRM.b, GGS.b], writes=[GGS.b])
            if debug:
                dma("sp", "dbg0", dbg_mod, MODT.ap, reads=[MODT.b], writes=[dbg_b[0]])

        def norm_tile_to_hT(SRC, XS, JN, hT, col0, avec, shift_cols, bankset, ti):
            ST = NST[ti % 2]
            op("act", lambda e: e.activation(out=JN.ap, in_=SRC.ap, func=AF.Square, accum_out=ST.ap[:, 0:1]),
               reads=[SRC.b, ST.b], writes=[JN.b, ST.b])
            op("act", lambda e: e.activation(out=ST.ap[:, 1:2], in_=ST.ap[:, 0:1], func=AF.Sqrt, scale=1.0 / D, bias=EPS),
               reads=[ST.b], writes=[ST.b])
            op("dve", lambda e: e.reciprocal(out=ST.ap[:, 2:3], in_=ST.ap[:, 1:2]), reads=[ST.b], writes=[ST.b])
            op("pool", lambda e: e.tensor_scalar(out=XS.ap, in0=SRC.ap, scalar1=ST.ap[:, 2:3], scalar2=None, op0=ALU.mult),
               reads=[SRC.b, XS.b, ST.b], writes=[XS.b])
            for kc in range(KC):
                bk = PS[bankset * 4 + kc // 4]
                sub = bk.ap.bitcast(BF16)[:, (kc % 4) * 128:(kc % 4) * 128 + 128]
                op("pe", lambda e, sub=sub, kc=kc: e.transpose(out=sub, in_=XS.ap[:, kc * 128:(kc + 1) * 128], identity=IDB1.ap),
                   reads=[XS.b, IDB1.b], writes=[bk.b])
            for kc in range(KC):
                bk = PS[bankset * 4 + kc // 4]
                sub = bk.ap.bitcast(BF16)[:, (kc % 4) * 128:(kc % 4) * 128 + 128]
                dst = hT.ap[:, kc, col0:col0 + 128]
                if False:
                    op("act", lambda e, sub=sub, dst=dst, kc=kc: e.activation(
                        out=dst, in_=sub, func=AF.Identity, scale=avec.ap[:, kc:kc + 1], bias=shift_cols[:, kc:kc + 1]),
                        reads=[bk.b, avec.b, MODT.b], writes=[hT.b])
                else:
                    op("dve", lambda e, sub=sub, dst=dst, kc=kc: e.tensor_scalar(
                        out=dst, in0=sub, scalar1=avec.ap[:, kc:kc + 1], scalar2=shift_cols[:, kc:kc + 1],
                        op0=ALU.mult, op1=ALU.add), reads=[bk.b, avec.b, MODT.b], writes=[hT.b])

        def rope_evac(bank, COS, SIN, ccol, dst_ap, dst_b, tset, n=512):
            T1, T2 = tset
            op("dve", lambda e: e.tensor_tensor(out=T1.ap[:, 0:n], in0=bank.ap[:, 0:n], in1=COS.ap[:, ccol:ccol + n], op=ALU.mult),
               reads=[bank.b, COS.b], writes=[T1.b])
            op("dve", lambda e: e.tensor_tensor(out=T2.ap[0:64, 0:n], in0=bank.ap[64:128, 0:n], in1=SIN.ap[64:128, ccol:ccol + n], op=ALU.mult),
               reads=[bank.b, SIN.b], writes=[T2.b])
            op("dve", lambda e: e.tensor_tensor(out=T2.ap[64:128, 0:n], in0=bank.ap[0:64, 0:n], in1=SIN.ap[0:64, ccol:ccol + n], op=ALU.mult),
               reads=[bank.b, SIN.b, T2.b], writes=[T2.b])
            op("pool", lambda e: e.tensor_tensor(out=dst_ap, in0=T1.ap[:, 0:n], in1=T2.ap[:, 0:n], op=ALU.add),
               reads=[T1.b, T2.b], writes=[dst_b])

        def phase_kv():
            HT = ar.alloc([128, KC, T], BF16)
            m1 = ar.top
            XS = [ar.alloc([128, D], F32) for _ in range(4)]
            JN = [ar.alloc([128, D], BF16) for _ in range(2)]
            for gt in range(NT):
                xs = XS[gt % 4]
                dma("sp", "xs%d" % (gt % 4), xs.ap, xb[gt * 128:(gt + 1) * 128, :], writes=[xs.b])
                norm_tile_to_hT(xs, JN[gt % 2], JN[gt % 2], HT, gt * 128, AM, MODT.ap[:, 0:16], gt % 2, gt)
            p.barrier()
            ar.top = m1
            WB = [ar.alloc([128, KC, 256], BF16) for _ in range(2)]
            TS = [(ar.alloc([128, 512], F32), ar.alloc([128, 512], F32)) for _ in range(2)]
            CS = [(ar.alloc([128, 512], F32), ar.alloc([128, 512], F32)) for _ in range(2)]
            KS = [ar.alloc([128, 512], BF16) for _ in range(4)]
            VS = [ar.alloc([128, 2, 129], BF16) for _ in range(4)]
            for v in VS:
                op("pool", lambda e, v=v: e.memset(v.ap, 1.0), writes=[v.b])
            n_ev = 0
            cs_n = 0
            for grp in range(8):
                wb = WB[grp % 2]
                dma("pool", "wb%d" % (grp % 2), wb.ap, w_in_r[grp].rearrange("p (kc c) -> p kc c", kc=KC), writes=[wb.b])
                roped = FM[2 * grp][1]
                for tc in range(8):
                    cs = None
                    if roped:
                        cs = CS[cs_n % 2]
                        dma("sp", "cs%da" % (cs_n % 2), cs[0].ap, cosk[:, tc * 512:(tc + 1) * 512], writes=[cs[0].b])
                        dma("sp", "cs%db" % (cs_n % 2), cs[1].ap, sink[:, tc * 512:(tc + 1) * 512], writes=[cs[1].b])
                        cs_n += 1
                    for cc in range(2):
                        idx = 2 * grp + cc
                        bank = PS[n_ev % 8]
                        for kc in range(KC):
                            op("pe", lambda e, bank=bank, kc=kc, cc=cc, wb=wb, tc=tc: e.matmul(
                                bank.ap, lhsT=wb.ap[:, kc, cc * 128:(cc + 1) * 128], rhs=HT.ap[:, kc, tc * 512:(tc + 1) * 512],
                                start=(kc == 0), stop=(kc == KC - 1)), reads=[wb.b, HT.b], writes=[bank.b])
                        ks = KS[n_ev % 4]
                        if roped:
                            rope_evac(bank, cs[0], cs[1], 0, ks.ap, ks.b, TS[n_ev % 2])
                        else:
                            op("act", lambda e, bank=bank, ks=ks: e.copy(out=ks.ap, in_=bank.ap), reads=[bank.b], writes=[ks.b])
                        dma("sp", "ks%d" % (n_ev % 4), KT[idx, :, tc * 512:(tc + 1) * 512], ks.ap, reads=[ks.b], writes=[ktb(idx, tc)])
                        n_ev += 1
            for grp in range(6):
                wb = WB[grp % 2]
                dma("pool", "wb%d" % (grp % 2), wb.ap, w_in_r[8 + grp].rearrange("p (kc c) -> p kc c", kc=KC), writes=[wb.b])
                for gt in range(NT):
                    bank = PS[n_ev % 8]
                    for kc in range(KC):
                        op("pe", lambda e, bank=bank, kc=kc, wb=wb, gt=gt: e.matmul(
                            bank.ap[:, 0:256], lhsT=HT.ap[:, kc, gt * 128:(gt + 1) * 128], rhs=wb.ap[:, kc, :],
                            start=(kc == 0), stop=(kc == KC - 1)), reads=[wb.b, HT.b], writes=[bank.b])
                    vs = VS[n_ev % 4]
                    src = bank.ap[:, 0:256].rearrange("p (h d) -> p h d", h=2)
                    if n_ev % 2 == 0:
                        op("act", lambda e, src=src, vs=vs: e.copy(out=vs.ap[:, :, 0:128], in_=src), reads=[bank.b], writes=[vs.b])
                    else:
                        op("dve", lambda e, src=src, vs=vs: e.tensor_copy(out=vs.ap[:, :, 0:128], in_=src), reads=[bank.b], writes=[vs.b])
                    dma("sp", "vs%d" % (n_ev % 4), VD[gt, :, grp * 258:(grp + 1) * 258], vs.ap.rearrange("p h d -> p (h d)"),
                        reads=[vs.b], writes=[vdb(gt, grp)])
                    n_ev += 1

        def phase_q(QT):
            HO = ar.alloc([128, KC, NL * 128], BF16)
            XS = [ar.alloc([128, D], F32) for _ in range(2)]
            JN = [ar.alloc([128, D], BF16) for _ in range(2)]
            WB = [ar.alloc([128, KC, 256], BF16) for _ in range(2)]
            WG = ar.alloc([128, KC, 24], BF16)
            TS = [(ar.alloc([128, 512], F32), ar.alloc([128, 512], F32)) for _ in range(2)]
            CQ = ar.alloc([128, NL * 128], F32)
            SQ = ar.alloc([128, NL * 128], F32)
            dma("sp", "q0", CQ.ap, cosq, writes=[CQ.b])
            dma("sp", "q1", SQ.ap, sinq, writes=[SQ.b])
            dma("pool", "q2", WG.ap, w_g_r.rearrange("p (kc c) -> p kc c", kc=KC), writes=[WG.b])
            for i in range(NL):
                xs = XS[i % 2]
                dma("sp", "xs%d" % (i % 2), xs.ap, xo[i * 128:(i + 1) * 128, :], writes=[xs.b])
                norm_tile_to_hT(xs, JN[i % 2], JN[i % 2], HO, i * 128, AM, MODT.ap[:, 0:16], i % 2, i)
            n_ev = 0
            for grp in range(8):
                wb = WB[grp % 2]
                dma("pool", "wb%d" % (grp % 2), wb.ap, w_in_r[14 + grp].rearrange("p (kc c) -> p kc c", kc=KC), writes=[wb.b])
                for half in range(2):
                    for cc in range(2):
                        head = 2 * grp + cc
                        bank = PS[n_ev % 8]
                        for kc in range(KC):
                            op("pe", lambda e, bank=bank, kc=kc, cc=cc, wb=wb, half=half: e.matmul(
                                bank.ap, lhsT=wb.ap[:, kc, cc * 128:(cc + 1) * 128], rhs=HO.ap[:, kc, half * 512:(half + 1) * 512],
                                start=(kc == 0), stop=(kc == KC - 1)), reads=[wb.b, HO.b], writes=[bank.b])
                        rope_evac(bank, CQ, SQ, half * 512, QT.ap[:, head, half * 512:(half + 1) * 512], QT.b, TS[n_ev % 2])
                        n_ev += 1
            for i in range(NL):
                bank = PS[n_ev % 8]
                for kc in range(KC):
                    op("pe", lambda e, bank=bank, kc=kc, i=i: e.matmul(
                        bank.ap[:, 0:24], lhsT=HO.ap[:, kc, i * 128:(i + 1) * 128], rhs=WG.ap[:, kc, :],
                        start=(kc == 0), stop=(kc == KC - 1)), reads=[WG.b, HO.b], writes=[bank.b])
                op("act", lambda e, bank=bank, i=i: e.activation(out=G.ap[:, i, :], in_=bank.ap[:, 0:24], func=AF.Sigmoid),
                   reads=[bank.b, G.b], writes=[G.b])
                n_ev += 1
            if debug:
                dma("sp", "dbg1", dbg_qt, QT.ap.rearrange("p h t -> p (h t)"), reads=[QT.b], writes=[dbg_b[1]])
                dma("sp", "dbg2", dbg_g, G.ap.rearrange("p i c -> p (i c)"), reads=[G.b], writes=[dbg_b[2]])

        def phase_c():
            KL = [ar.alloc([128, T], BF16) for _ in range(2)]
            W1 = ar.alloc([128, 32, 128], BF16)
            W2 = ar.alloc([128, 128], BF16)
            PT_ = ar.alloc([128, 32], BF16)
            PC = ar.alloc([128, 1], F32)
            HS = ar.alloc([128, 256], BF16)
            KM32 = ar.alloc([128, 16], F32)
            op("pool", lambda e: e.memset(HS.ap, 0.0), writes=[HS.b])
            op("pool", lambda e: e.memset(ZR.ap, 0.0), writes=[ZR.b])
            op("pool", lambda e: e.memset(VCP.ap, 1.0), writes=[VCP.b])
            nl = 0
            for typ in range(2):
                dma("pool", "c3", W1.ap, cw1[typ].rearrange("d (l h) -> d l h", l=32), writes=[W1.b])
                dma("pool", "c4", W2.ap, cw2[typ], writes=[W2.b])
                dma("pool", "c5", PT_.ap, cposT[typ], writes=[PT_.b])
                bank = PS[0]
                for l in range(32):
                    op("pe", lambda e, l=l, bank=bank: e.matmul(bank.ap[:, 0:1], lhsT=W1.ap[:, l, :], rhs=PT_.ap[:, l:l + 1],
                                                               start=(l == 0), stop=(l == 31)), reads=[W1.b, PT_.b], writes=[bank.b])
                op("dve", lambda e, bank=bank: e.tensor_copy(out=PC.ap, in_=bank.ap[:, 0:1]), reads=[bank.b], writes=[PC.b])
                for g in range(2):
                    kl = KL[nl % 2]
                    idx = 2 * typ + g
                    dma("sp", "kl%d" % (nl % 2), kl.ap, KT[idx], reads=[ktb(idx, tc) for tc in range(8)], writes=[kl.b])
                    nl += 1
                    kl3 = kl.ap.rearrange("p (n s) -> p n s", s=16)
                    bank = PS[1 + g]
                    for l in range(32):
                        a = l // 16
                        op("pe", lambda e, l=l, a=a, bank=bank, kl3=kl3: e.matmul(
                            bank.ap[:, 0:255], lhsT=W1.ap[:, l, :], rhs=kl3[:, a:a + 255, l % 16],
                            start=(l == 0), stop=(l == 31)), reads=[W1.b, kl.b], writes=[bank.b])
                    op("act", lambda e, bank=bank: e.activation(out=HS.ap[:, 0:255], in_=bank.ap[:, 0:255], func=AF.Silu, bias=PC.ap[:, 0:1]),
                       reads=[bank.b, PC.b], writes=[HS.b])
                    bank2 = PS[3 + g]
                    if typ == 0:
                        op("pe", lambda e, bank2=bank2: e.matmul(bank2.ap[:, 0:256], lhsT=W2.ap, rhs=HS.ap, start=True, stop=True),
                           reads=[W2.b, HS.b], writes=[bank2.b])
                        op("dve", lambda e, bank2=bank2, g=g: e.tensor_copy(out=KCT.ap[:, g, :], in_=bank2.ap[:, 0:256]),
                           reads=[bank2.b, KCT.b], writes=[KCT.b])
                    else:
                        for ct in range(2):
                            op("pe", lambda e, bank2=bank2, ct=ct: e.matmul(bank2.ap[:, ct * 128:(ct + 1) * 128], lhsT=HS.ap[:, ct * 128:(ct + 1) * 128],
                                                                            rhs=W2.ap, start=True, stop=True), reads=[W2.b, HS.b], writes=[bank2.b])
                        op("dve", lambda e, bank2=bank2, g=g: e.tensor_copy(
                            out=VCP.ap[:, g, :, 0:128], in_=bank2.ap[:, 0:256].rearrange("p (c d) -> p c d", c=2)),
                            reads=[bank2.b, VCP.b], writes=[VCP.b])
            for h in range(8):
                kl = KL[nl % 2]
                dma("sp", "kl%d" % (nl % 2), kl.ap, KT[8 + h], reads=[ktb(8 + h, tc) for tc in range(8)], writes=[kl.b])
                nl += 1
                op("dve", lambda e, kl=kl: e.tensor_reduce(out=KM32.ap, in_=kl.ap.rearrange("p (b k) -> p b k", k=256), axis=AX.X, op=ALU.add),
                   reads=[kl.b], writes=[KM32.b])
                op("act", lambda e, h=h: e.activation(out=KMT.ap[:, h, :], in_=KM32.ap, func=AF.Copy, scale=1.0 / 256),
                   reads=[KM32.b, KMT.b], writes=[KMT.b])
            if debug:
                dma("sp", "dbg3", dbg_kc, KCT.ap.rearrange("p g c -> p (g c)"), reads=[KCT.b], writes=[dbg_b[3]])
                dma("sp", "dbg4", dbg_vc, VCP.ap.rearrange("p g c d -> p (g c d)"), reads=[VCP.b], writes=[dbg_b[4]])
                dma("sp", "dbg5", dbg_km, KMT.ap.rearrange("p h n -> p (h n)"), reads=[KMT.b], writes=[dbg_b[5]])

        def phase_a(QT, OT):
            CM = ar.alloc([128, 4, 128], BF16)
            WM = ar.alloc([128, 8, 128], BF16)
            CMPF = [ar.alloc([128, 256], F32) for _ in range(2)]
            CMPB = ar.alloc([128, NL, 256], BF16)
            NSAB = ar.alloc([128, NL, 64], F32)
            MOBAB = ar.alloc([128, NL, 16], F32)
            MOBAV = ar.alloc([128, NL, 16], F32)
            dma("sp", "a0", CM.ap, cmask_d.rearrange("p (a b) -> p a b", a=4), writes=[CM.b])
            dma("sp", "a1", WM.ap, wmask_d.rearrange("p (a b) -> p a b", a=8), writes=[WM.b])
            dma("sp", "a3", CMPB.ap, cmpb_d.rearrange("p (a b) -> p a b", a=NL), writes=[CMPB.b])
            dma("sp", "a4", NSAB.ap, nsab_d.rearrange("p (a b) -> p a b", a=NL), writes=[NSAB.b])
            dma("sp", "a5", MOBAB.ap, mobab_d.rearrange("p (a b) -> p a b", a=NL), writes=[MOBAB.b])
            dma("sp", "a6", MOBAV.ap, mobav_d.rearrange("p (a b) -> p a b", a=NL), writes=[MOBAV.b])
            KA = [ar.alloc([128, T], BF16) for _ in range(4)]
            VA = [ar.alloc([128, 8, 4 * 129], BF16) for _ in range(4)]
            MEXP = ar.alloc([128, T], BF16)
            PTB = [ar.alloc([128, 512], BF16) for _ in range(3)]
            SM = ar.alloc([128, 4, 256], F32)
            PACC = ar.alloc([128, 256], F32)
            IMP = ar.alloc([128, 64], F32)
            SCW = ar.alloc([128, 64], F32)
            MSEL = ar.alloc([128, 64], F32)
            M8 = ar.alloc([128, 16], F32)
            SS = ar.alloc([128, 16], F32)
            OEV = [ar.alloc([128, 4, 129], F32) for _ in range(3)]
            CF = ar.alloc([128, 3, 4], F32)
            OTOK = ar.alloc([128, 4, 128], F32)
            GM = ar.alloc([128, 4, 16], F32)
            MB = ar.alloc([128, 4, 16], F32)
            MBB = ar.alloc([128, 4, 16], BF16)
            st_n = [0]
            oset_n = [0]

            def oset():
                k = oset_n[0] % 2
                oset_n[0] += 1
                return (PS[3 + 2 * k], PS[4 + 2 * k])

            def oreg(os_, h):
                bk = os_[h // 2]
                return bk, bk.ap[:, (h % 2) * 129:(h % 2) * 129 + 129]

            def evac_o(os_, dst):
                for k in range(2):
                    src = os_[k].ap[:, 0:258].rearrange("p (h d) -> p h d", h=2)
                    if k == 0:
                        op("act", lambda e, src=src: e.copy(out=dst.ap[:, 0:2, :], in_=src), reads=[os_[k].b, dst.b], writes=[dst.b])
                    else:
                        op("dve", lambda e, src=src: e.tensor_copy(out=dst.ap[:, 2:4, :], in_=src), reads=[os_[k].b, dst.b], writes=[dst.b])

            def attn_step(kq_pairs, mask_list, v_aps, v_b, os_, first, last):
                bk = PS[st_n[0] % 3]
                pt = PTB[st_n[0] % 3]
                st_n[0] += 1
                for (c0, ncol, kl, qr, rd), masks in zip(kq_pairs, mask_list):
                    outap = bk.ap[:, c0:c0 + ncol]
                    op("pe", lambda e, outap=outap, kl=kl, qr=qr: e.matmul(outap, lhsT=kl, rhs=qr, start=True, stop=False),
                       reads=rd, writes=[bk.b])
                    for mi, (ml, mr, mrd) in enumerate(masks):
                        op("pe", lambda e, outap=outap, ml=ml, mr=mr, lastm=(mi == len(masks) - 1): e.matmul(
                            outap, lhsT=ml, rhs=mr, start=False, stop=lastm), reads=mrd, writes=[bk.b])
                op("act", lambda e, bk=bk, pt=pt: e.activation(out=pt.ap, in_=bk.ap, func=AF.Exp), reads=[bk.b], writes=[pt.b])
                if first:
                    for k in range(2):
                        op("pe", lambda e, k=k: e.matmul(os_[k].ap[:, 0:258], lhsT=ZR.ap[:, 0:128], rhs=ZR.ap, start=True, stop=False),
                           reads=[ZR.b], writes=[os_[k].b])
                for h in range(4):
                    obk, oap = oreg(os_, h)
                    op("pe", lambda e, oap=oap, pt=pt, h=h: e.matmul(oap, lhsT=pt.ap[:, h * 128:(h + 1) * 128], rhs=v_aps[h],
                                                                     start=False, stop=last), reads=[pt.b, v_b], writes=[obk.b])

            def finalize(osrcs, coefs_fn, feat0, i):
                nb = len(osrcs)
                for bi, oe in enumerate(osrcs):
                    op("dve", lambda e, oe=oe, bi=bi: e.tensor_scalar(out=CF.ap[:, bi, :], in0=oe.ap[:, :, 128], scalar1=1e-30, scalar2=None, op0=ALU.max),
                       reads=[oe.b, CF.b], writes=[CF.b])
                    op("dve", lambda e, bi=bi: e.reciprocal(out=CF.ap[:, bi, :], in_=CF.ap[:, bi, :]), reads=[CF.b], writes=[CF.b])
                    gap = coefs_fn(bi)
                    if gap is not None:
                        op("dve", lambda e, bi=bi, gap=gap: e.tensor_tensor(out=CF.ap[:, bi, :], in0=CF.ap[:, bi, :], in1=gap, op=ALU.mult),
                           reads=[CF.b, G.b], writes=[CF.b])
                for h in range(4):
                    op("dve", lambda e, h=h: e.tensor_scalar(out=OTOK.ap[:, h, :], in0=osrcs[0].ap[:, h, 0:128], scalar1=CF.ap[:, 0, h:h + 1],
                                                            scalar2=None, op0=ALU.mult), reads=[osrcs[0].b, CF.b, OTOK.b], writes=[OTOK.b])
                    for bi in range(1, nb):
                        op("dve", lambda e, h=h, bi=bi: e.scalar_tensor_tensor(
                            out=OTOK.ap[:, h, :], in0=osrcs[bi].ap[:, h, 0:128], scalar=CF.ap[:, bi, h:h + 1], in1=OTOK.ap[:, h, :],
                            op0=ALU.mult, op1=ALU.add), reads=[osrcs[bi].b, CF.b, OTOK.b], writes=[OTOK.b])
                bk = PS[7]
                for h in range(4):
                    op("pe", lambda e, h=h, bk=bk: e.transpose(out=bk.ap[:, h * 128:(h + 1) * 128], in_=OTOK.ap[:, h, :], identity=IDF.ap),
                       reads=[OTOK.b, IDF.b], writes=[bk.b])
                op("act", lambda e, bk=bk: e.copy(out=OT.ap[:, feat0:feat0 + 4, i * 128:(i + 1) * 128],
                                                  in_=bk.ap.rearrange("p (h q) -> p h q", h=4)), reads=[bk.b, OT.b], writes=[OT.b])

            for k in range(4):
                dma("sp", "ka%d" % k, KA[k].ap, KT[4 + k], reads=[ktb(4 + k, tc) for tc in range(8)], writes=[KA[k].b])
            for q4 in range(4):
                dma("sp", "va%d" % q4, VA[q4].ap,
                    VD[8 * q4:8 * q4 + 8, :, 0:516].rearrange("t p c -> p t c"),
                    reads=[vdb(gt, grp) for gt in range(8 * q4, 8 * q4 + 8) for grp in range(2)], writes=[VA[q4].b])
            for i in range(NL):
                nk = 4 * i + 4
                cmpf = CMPF[i % 2]
                dma("sp", "cf%d" % (i % 2), cmpf.ap, cmpf_d[:, i * 256:(i + 1) * 256], writes=[cmpf.b])
                for g in range(2):
                    qr4 = QT.ap[:, 4 * g:4 * g + 4, i * 128:(i + 1) * 128]
                    os_ = oset()
                    for h in range(4):
                        bk = os_[h // 2]
                        op("pe", lambda e, bk=bk, h=h, g=g, i=i: e.matmul(
                            bk.ap[:, (h % 2) * 256:(h % 2) * 256 + 256], lhsT=QT.ap[:, 4 * g + h, i * 128:(i + 1) * 128],
                            rhs=KCT.ap[:, g, :], start=True, stop=True), reads=[QT.b, KCT.b], writes=[bk.b])
                    for k in range(2):
                        op("dve", lambda e, k=k, os_=os_, cmpf=cmpf: e.tensor_tensor(
                            out=SM.ap[:, 2 * k:2 * k + 2, :], in0=os_[k].ap.rearrange("p (h c) -> p h c", h=2),
                            in1=cmpf.ap.unsqueeze(1).to_broadcast([128, 2, 256]), op=ALU.add),
                            reads=[os_[k].b, cmpf.b, SM.b], writes=[SM.b])
                    for h in range(4):
                        op("act", lambda e, h=h: e.activation(out=SM.ap[:, h, :], in_=SM.ap[:, h, :], func=AF.Exp, accum_out=SS.ap[:, h:h + 1]),
                           reads=[SM.b, SS.b], writes=[SM.b, SS.b])
                    op("dve", lambda e: e.tensor_scalar(out=SS.ap[:, 4:8], in0=SS.ap[:, 0:4], scalar1=1e-30, scalar2=None, op0=ALU.max),
                       reads=[SS.b], writes=[SS.b])
                    op("dve", lambda e: e.reciprocal(out=SS.ap[:, 8:12], in_=SS.ap[:, 4:8]), reads=[SS.b], writes=[SS.b])
                    op("dve", lambda e: e.tensor_scalar(out=PACC.ap, in0=SM.ap[:, 0, :], scalar1=SS.ap[:, 8:9], scalar2=None, op0=ALU.mult),
                       reads=[SM.b, SS.b], writes=[PACC.b])
                    for h in range(1, 4):
                        op("dve", lambda e, h=h: e.scalar_tensor_tensor(out=PACC.ap, in0=SM.ap[:, h, :], scalar=SS.ap[:, 8 + h:9 + h], in1=PACC.ap,
                                                                        op0=ALU.mult, op1=ALU.add), reads=[SM.b, SS.b, PACC.b], writes=[PACC.b])
                    P3 = PACC.ap.rearrange("p (s r) -> p s r", r=4)
                    op("dve", lambda e, P3=P3: e.tensor_tensor(out=IMP.ap, in0=P3[:, :, 0], in1=P3[:, :, 1], op=ALU.add), reads=[PACC.b], writes=[IMP.b])
                    op("dve", lambda e, P3=P3: e.tensor_tensor(out=IMP.ap, in0=IMP.ap, in1=P3[:, :, 2], op=ALU.add), reads=[PACC.b, IMP.b], writes=[IMP.b])
                    op("dve", lambda e, P3=P3: e.scalar_tensor_tensor(out=IMP.ap, in0=P3[:, :, 3], scalar=0.5, in1=IMP.ap, op0=ALU.mult, op1=ALU.add),
                       reads=[PACC.b, IMP.b], writes=[IMP.b])
                    op("dve", lambda e, P3=P3: e.scalar_tensor_tensor(out=IMP.ap[:, 1:64], in0=P3[:, 0:63, 3], scalar=0.5, in1=IMP.ap[:, 1:64],
                                                                      op0=ALU.mult, op1=ALU.add), reads=[PACC.b, IMP.b], writes=[IMP.b])
                    op("dve", lambda e, i=i: e.tensor_tensor(out=IMP.ap, in0=IMP.ap, in1=NSAB.ap[:, i, :], op=ALU.add),
                       reads=[IMP.b, NSAB.b], writes=[IMP.b])
                    op("dve", lambda e: e.max(out=M8.ap[:, 0:8], in_=IMP.ap), reads=[IMP.b, M8.b], writes=[M8.b])
                    op("dve", lambda e: e.match_replace(out=SCW.ap, in_to_replace=M8.ap[:, 0:8], in_values=IMP.ap, imm_value=-30000.0),
                       reads=[IMP.b, M8.b], writes=[SCW.b])
                    op("dve", lambda e: e.max(out=M8.ap[:, 8:16], in_=SCW.ap), reads=[SCW.b, M8.b], writes=[M8.b])
                    op("dve", lambda e: e.tensor_scalar(out=MSEL.ap, in0=IMP.ap, scalar1=M8.ap[:, 15:16], scalar2=1.0, op0=ALU.is_ge, op1=ALU.subtract),
                       reads=[IMP.b, M8.b], writes=[MSEL.b])
                    op("dve", lambda e, nk=nk: e.tensor_copy(
                        out=MEXP.ap[:, 0:nk * 128].rearrange("p (s k) -> p s k", k=64),
                        in_=MSEL.ap[:, 0:2 * nk].unsqueeze(2).to_broadcast([128, 2 * nk, 64])), reads=[MSEL.b], writes=[MEXP.b])
                    os_ = oset()
                    for ct in range(2):
                        attn_step([(0, 512, KCT.ap[:, g, ct * 128:(ct + 1) * 128], qr4, [KCT.b, QT.b])],
                                  [[(CMPB.ap[:, i, ct * 128:(ct + 1) * 128], IDB4.ap, [CMPB.b, IDB4.b])]],
                                  [VCP.ap[:, g, ct, :]] * 4, VCP.b, os_, ct == 0, ct == 1)
                    evac_o(os_, OEV[0])
                    os_ = oset()
                    for kt in range(nk):
                        masks = [(MEXP.ap[:, kt * 128:(kt + 1) * 128], IDB4.ap, [MEXP.b, IDB4.b])]
                        if kt >= 4 * i:
                            masks.append((CM.ap[:, kt - 4 * i, :], IDB4.ap, [CM.b, IDB4.b]))
                        attn_step([(0, 512, KA[g].ap[:, kt * 128:(kt + 1) * 128], qr4, [KA[g].b, QT.b])], [masks],
                                  [VA[kt // 8].ap[:, kt % 8, g * 129:(g + 1) * 129]] * 4, VA[kt // 8].b, os_, kt == 0, kt == nk - 1)
                    evac_o(os_, OEV[1])
                    os_ = oset()
                    k0 = max(0, 4 * i - 4)
                    for kt in range(k0, nk):
                        masks = [(WM.ap[:, kt - (4 * i - 4), :], IDB4.ap, [WM.b, IDB4.b])]
                        attn_step([(0, 512, KA[2 + g].ap[:, kt * 128:(kt + 1) * 128], qr4, [KA[2 + g].b, QT.b])], [masks],
                                  [VA[kt // 8].ap[:, kt % 8, (2 + g) * 129:(3 + g) * 129]] * 4, VA[kt // 8].b, os_, kt == k0, kt == nk - 1)
                    evac_o(os_, OEV[2])
                    G3 = G.ap[:, i, :].rearrange("p (h b) -> p h b", b=3)
                    finalize(OEV, lambda bi, G3=G3, g=g: G3[:, 4 * g:4 * g + 4, bi], 4 * g, i)
            for r in range(2):
                for k in range(4):
                    dma("sp", "ka%d" % k, KA[k].ap, KT[8 + 4 * r + k], reads=[ktb(8 + 4 * r + k, tc) for tc in range(8)], writes=[KA[k].b])
                for q4 in range(4):
                    dma("sp", "va%d" % q4, VA[q4].ap,
                        VD[8 * q4:8 * q4 + 8, :, (4 + 4 * r) * 129:(8 + 4 * r) * 129].rearrange("t p c -> p t c"),
                        reads=[vdb(gt, grp) for gt in range(8 * q4, 8 * q4 + 8) for grp in range(2 + 2 * r, 4 + 2 * r)], writes=[VA[q4].b])
                for i in range(NL):
                    nk = 4 * i + 4
                    bk7 = PS[7]
                    for h in range(4):
                        op("pe", lambda e, h=h, r=r, i=i: e.matmul(bk7.ap[:, h * 16:(h + 1) * 16], lhsT=QT.ap[:, 8 + 4 * r + h, i * 128:(i + 1) * 128],
                                                                   rhs=KMT.ap[:, 4 * r + h, :], start=True, stop=True), reads=[QT.b, KMT.b], writes=[bk7.b])
                    op("dve", lambda e, i=i: e.tensor_tensor(out=GM.ap, in0=bk7.ap[:, 0:64].rearrange("p (h n) -> p h n", h=4),
                                                             in1=MOBAB.ap[:, i:i + 1, :].to_broadcast([128, 4, 16]), op=ALU.add),
                       reads=[bk7.b, MOBAB.b], writes=[GM.b])
                    for h in range(4):
                        op("dve", lambda e, h=h: e.max(out=M8.ap[:, 0:8], in_=GM.ap[:, h, :]), reads=[GM.b, M8.b], writes=[M8.b])
                        op("dve", lambda e, h=h: e.tensor_scalar(out=MB.ap[:, h, :], in0=GM.ap[:, h, :], scalar1=M8.ap[:, 2:3], scalar2=1.0,
                                                                op0=ALU.is_ge, op1=ALU.subtract), reads=[GM.b, M8.b, MB.b], writes=[MB.b])
                    op("dve", lambda e, i=i: e.tensor_tensor(out=MBB.ap, in0=MB.ap, in1=MOBAV.ap[:, i:i + 1, :].to_broadcast([128, 4, 16]), op=ALU.mult),
                       reads=[MB.b, MOBAV.b], writes=[MBB.b])
                    os_ = oset()
                    for kt in range(nk):
                        pairs = []
                        masks_all = []
                        for h in range(4):
                            pairs.append((h * 128, 128, KA[h].ap[:, kt * 128:(kt + 1) * 128], QT.ap[:, 8 + 4 * r + h, i * 128:(i + 1) * 128], [KA[h].b, QT.b]))
                            ms = [(MBB.ap[:, h, kt // 2:kt // 2 + 1].to_broadcast([128, 128]), IDB4.ap[:, 0:128], [MBB.b, IDB4.b])]
                            if kt >= 4 * i:
                                ms.append((CM.ap[:, kt - 4 * i, :], IDB4.ap[:, 0:128], [CM.b, IDB4.b]))
                            masks_all.append(ms)
                        attn_step(pairs, masks_all, [VA[kt // 8].ap[:, kt % 8, h * 129:(h + 1) * 129] for h in range(4)], VA[kt // 8].b, os_, kt == 0, kt == nk - 1)
                    evac_o(os_, OEV[0])
                    finalize([OEV[0]], lambda bi: None, 8 + 4 * r, i)
            if debug:
                dma("sp", "dbg6", dbg_ot, OT.ap.rearrange("p h t -> p (h t)"), reads=[OT.b], writes=[dbg_b[6]])

        def bcast_vec(src_ap, src_b, dst):
            for c4 in range(4):
                bk = PS[c4]
                for cc in range(4):
                    c = 4 * c4 + cc
                    op("pe", lambda e, bk=bk, cc=cc, c=c: e.matmul(bk.ap[:, cc * 128:(cc + 1) * 128], lhsT=src_ap[:, c:c + 1].to_broadcast([128, 128]),
                                                                   rhs=IDF.ap, start=True, stop=True), reads=[src_b, IDF.b], writes=[bk.b])
                op("dve", lambda e, bk=bk, c4=c4: e.tensor_copy(out=dst.ap[:, c4 * 512:(c4 + 1) * 512], in_=bk.ap), reads=[bk.b, dst.b], writes=[dst.b])

        def rstd_from_ss(ss_ap, dst_ap, b):
            op("act", lambda e: e.activation(out=dst_ap, in_=ss_ap, func=AF.Sqrt, scale=1.0 / D, bias=EPS), reads=[b], writes=[b])
            op("dve", lambda e: e.reciprocal(out=dst_ap, in_=dst_ap), reads=[b], writes=[b])

        def phase_o(OT, H2):
            WO = ar.alloc([128, KC, D], BF16)
            GGM = ar.alloc([128, D], F32)
            XS = [ar.alloc([128, D], F32) for _ in range(2)]
            X1 = ar.alloc([128, D], F32)
            JN = ar.alloc([128, D], BF16)
            ST = [ar.alloc([128, 8], F32) for _ in range(2)]
            for cg in range(4):
                dma("pool", "wo%d" % cg, WO.ap[:, :, cg * 512:(cg + 1) * 512],
                    w_o_r[cg].rearrange("p (kc c) -> p kc c", kc=KC), writes=[WO.b])
            bcast_vec(GGS.ap[:, 0, :], GGS.b, GGM)
            for i in range(NL):
                bs = 4 * (i % 2)
                xs = XS[i % 2]
                x1 = X1
                stt = ST[i % 2]
                dma("sp", "xs%d" % (i % 2), xs.ap, xo[i * 128:(i + 1) * 128, :], writes=[xs.b])
                for cg in range(4):
                    bk = PS[bs + cg]
                    for kc in range(KC):
                        op("pe", lambda e, bk=bk, kc=kc, cg=cg, i=i: e.matmul(bk.ap, lhsT=OT.ap[:, kc, i * 128:(i + 1) * 128],
                                                                             rhs=WO.ap[:, kc, cg * 512:(cg + 1) * 512], start=(kc == 0), stop=(kc == KC - 1)),
                           reads=[OT.b, WO.b], writes=[bk.b])
                    op("act", lambda e, bk=bk, cg=cg, stt=stt: e.activation(out=JN.ap[:, 0:512], in_=bk.ap, func=AF.Square, accum_out=stt.ap[:, cg:cg + 1]),
                       reads=[bk.b, stt.b, JN.b], writes=[JN.b, stt.b])
                op("dve", lambda e, stt=stt: e.tensor_reduce(out=stt.ap[:, 4:5], in_=stt.ap[:, 0:4], axis=AX.X, op=ALU.add), reads=[stt.b], writes=[stt.b])
                rstd_from_ss(stt.ap[:, 4:5], stt.ap[:, 5:6], stt.b)
                for cg in range(4):
                    bk = PS[bs + cg]
                    op("dve", lambda e, bk=bk, cg=cg, stt=stt, x1=x1: e.scalar_tensor_tensor(
                        out=x1.ap[:, cg * 512:(cg + 1) * 512], in0=bk.ap, scalar=stt.ap[:, 5:6], in1=GGM.ap[:, cg * 512:(cg + 1) * 512],
                        op0=ALU.mult, op1=ALU.mult), reads=[bk.b, stt.b, GGM.b, x1.b], writes=[x1.b])
                op("pool", lambda e, x1=x1, xs=xs: e.tensor_tensor(out=x1.ap, in0=x1.ap, in1=xs.ap, op=ALU.add), reads=[x1.b, xs.b], writes=[x1.b])
                dma("sp", "x1s", X1S[i * 128:(i + 1) * 128, :], x1.ap, reads=[x1.b], writes=[x1_b[i]])
                norm_tile_to_hT(x1, JN, JN, H2, i * 128, AFF, MODT.ap[:, 48:64], (i + 1) % 2, i)

        def phase_f(H2):
            UT = ar.alloc([128, 64, NL * 128], BF16)
            WU = [ar.alloc([128, KC, 128], BF16) for _ in range(2)]
            RR = ar.alloc([128, 512], F32)
            n_ev = 0
            for fc in range(64):
                wu = WU[fc % 2]
                dma("pool", "wu%d" % (fc % 2), wu.ap, w_up_r[fc].rearrange("p (kc c) -> p kc c", kc=KC), writes=[wu.b])
                for half in range(2):
                    bk = PS[n_ev % 8]
                    for kc in range(KC):
                        op("pe", lambda e, bk=bk, kc=kc, wu=wu, half=half: e.matmul(bk.ap, lhsT=wu.ap[:, kc, :], rhs=H2.ap[:, kc, half * 512:(half + 1) * 512],
                                                                                  start=(kc == 0), stop=(kc == KC - 1)), reads=[wu.b, H2.b], writes=[bk.b])
                    op("dve", lambda e, bk=bk: e.tensor_scalar(out=RR.ap, in0=bk.ap, scalar1=0.0, scalar2=None, op0=ALU.max), reads=[bk.b, RR.b], writes=[RR.b])
                    op("pool", lambda e, fc=fc, half=half: e.tensor_tensor(out=UT.ap[:, fc, half * 512:(half + 1) * 512], in0=RR.ap, in1=RR.ap, op=ALU.mult),
                       reads=[RR.b, UT.b], writes=[UT.b])
                    n_ev += 1
            p.barrier()
            if stop_after == "f_up":
                return
            ar.top = SLOT1
            WD = [ar.alloc([128, 8, 512], BF16) for _ in range(2)]
            FSB = [ar.alloc([128, 512], F32) for _ in range(2)]
            JN = ar.alloc([128, 512], BF16)
            assert ar.top <= SLOT2
            n_w = 0
            n_f = 0
            for cg in range(4):
                for kg in range(8):
                    wd = WD[n_w % 2]
                    dma("pool", "wd%d" % (n_w % 2), wd.ap,
                        w_down_r[cg * 8 + kg].rearrange("p (kk c) -> p kk c", kk=8), writes=[wd.b])
                    n_w += 1
                    for kk in range(8):
                        kc = kg * 8 + kk
                        for i in range(NL):
                            bk = PS[i]
                            op("pe", lambda e, bk=bk, kc=kc, kk=kk, wd=wd, i=i: e.matmul(bk.ap, lhsT=UT.ap[:, kc, i * 128:(i + 1) * 128], rhs=wd.ap[:, kk, :],
                                                                                       start=(kc == 0), stop=(kc == 63)), reads=[UT.b, wd.b], writes=[bk.b])
                for i in range(NL):
                    bk = PS[i]
                    fsb_ = FSB[n_f % 2]
                    op("act", lambda e, bk=bk, i=i, cg=cg: e.activation(out=JN.ap, in_=bk.ap, func=AF.Square, accum_out=SSF.ap[:, i, cg:cg + 1]),
                       reads=[bk.b, SSF.b, JN.b], writes=[JN.b, SSF.b])
                    op("dve", lambda e, bk=bk, fsb_=fsb_: e.tensor_copy(out=fsb_.ap, in_=bk.ap), reads=[bk.b, fsb_.b, SSF.b], writes=[fsb_.b])
                    dma("sp", "fs%d" % (n_f % 2), FS[i * 128:(i + 1) * 128, cg * 512:(cg + 1) * 512], fsb_.ap, reads=[fsb_.b], writes=[fsb(i, cg)])
                    n_f += 1
            p.barrier()
            if stop_after == "f_down":
                return
            ar.top = SLOT1
            GGF = ar.alloc([128, D], F32)
            FT = [ar.alloc([128, D], F32) for _ in range(2)]
            XT = [ar.alloc([128, D], F32) for _ in range(2)]
            ST = ar.alloc([128, NL, 2], F32)
            bcast_vec(GGS.ap[:, 1, :], GGS.b, GGF)
            for i in range(NL):
                ft = FT[i % 2]
                xt = XT[i % 2]
                dma("sp", "ft%d" % (i % 2), ft.ap, FS[i * 128:(i + 1) * 128, :], reads=[fsb(i, cg) for cg in range(4)], writes=[ft.b])
                dma("sp", "xt%d" % (i % 2), xt.ap, X1S[i * 128:(i + 1) * 128, :], reads=[x1_b[i]], writes=[xt.b])
                op("dve", lambda e, i=i: e.tensor_reduce(out=ST.ap[:, i, 0:1], in_=SSF.ap[:, i, :], axis=AX.X, op=ALU.add), reads=[SSF.b, ST.b], writes=[ST.b])
                rstd_from_ss(ST.ap[:, i, 0:1], ST.ap[:, i, 1:2], ST.b)
                op("dve", lambda e, i=i, ft=ft: e.scalar_tensor_tensor(out=ft.ap, in0=ft.ap, scalar=ST.ap[:, i, 1:2], in1=GGF.ap, op0=ALU.mult, op1=ALU.mult),
                   reads=[ft.b, ST.b, GGF.b], writes=[ft.b])
                op("pool", lambda e, ft=ft, xt=xt: e.tensor_tensor(out=ft.ap, in0=ft.ap, in1=xt.ap, op=ALU.add), reads=[ft.b, xt.b], writes=[ft.b])
                dma("sp", "out%d" % (i % 2), out[i * 128:(i + 1) * 128, :], ft.ap, reads=[ft.b], writes=[out_b[i]])

        def run_all():
            phase_mod()
            p.barrier()
            if stop_after == "mod":
                return
            ar.top = persist_mark
            if "kv" not in skip:
                phase_kv()
            p.barrier()
            if stop_after == "kv":
                return
            ar.top = SLOT1
            QT = ar.alloc([128, 16, NL * 128], BF16)
            assert ar.top == SLOT2
            ar.top = SLOT3
            phase_q(QT)
            p.barrier()
            if stop_after == "q":
                return
            ar.top = SLOT3
            phase_c()
            p.barrier()
            if stop_after == "c":
                return
            ar.top = SLOT2
            OT = ar.alloc([128, 16, NL * 128], BF16)
            assert ar.top == PB
            ar.top = SLOT3
            if "a" not in skip:
                phase_a(QT, OT)
            p.barrier()
            if stop_after == "a":
                return
            ar.top = SLOT1
            H2 = ar.alloc([128, KC, NL * 128], BF16)
            ar.top = PB
            phase_o(OT, H2)
            p.barrier()
            if stop_after == "o":
                return
            ar.top = SLOT2
            phase_f(H2)

        run_all()
        fin = list(out_b)
        if debug:
            fin += dbg_b + list(kt_b.values()) + list(vd_b.values()) + x1_b + list(fs_b.values())
        p.final_wait("sp", fin)
        p.emit()
    return nc


def _tables():
    inv = (10000.0 ** (-np.arange(0, 128, 2, dtype=np.float32) / np.float32(128))).astype(np.float32)
    pos = np.arange(T, dtype=np.float32)
    ang = (pos[:, None] * inv[None, :]).astype(np.float32)
    cos = np.cos(ang).astype(np.float32).T
    sin = np.sin(ang).astype(np.float32).T
    cosk = np.concatenate([cos, cos], 0)
    sink = np.concatenate([sin, -sin], 0)
    return np.ascontiguousarray(cosk), np.ascontiguousarray(sink)


def _core_tables(j, cosk, sink):
    bf = ml_dtypes.bfloat16
    own = np.concatenate([np.arange(128 * (4 * i + j), 128 * (4 * i + j) + 128) for i in range(NL)])
    sc = np.float32(128 ** -0.5)
    cosq = np.ascontiguousarray(cosk[:, own] * sc).astype(np.float32)
    sinq = np.ascontiguousarray(sink[:, own] * sc).astype(np.float32)
    q = np.arange(128)[:, None]
    k = np.arange(128)[None, :]
    cm = np.zeros((128, 4, 128), np.float32)
    for jp in range(4):
        if jp == j:
            cm[:, jp, :] = np.where(k <= q, 0.0, -1.0)
        elif jp > j:
            cm[:, jp, :] = -1.0
    wm = np.zeros((128, 8, 128), np.float32)
    for jj in range(8):
        dist = 128 * (j + 4 - jj) + q - k
        wm[:, jj, :] = np.where((dist >= 0) & (dist < 512), 0.0, -1.0)
    cmpf = np.zeros((128, NL, 256), np.float32)
    nsab = np.zeros((128, NL, 64), np.float32)
    mobab = np.zeros((128, NL, 16), np.float32)
    mobav = np.zeros((128, NL, 16), np.float32)
    c = np.arange(256)[None, :]
    s = np.arange(64)[None, :]
    n = np.arange(16)[None, :]
    for i in range(NL):
        tq = 128 * (4 * i + j) + np.arange(128)[:, None]
        vis = (16 * c + 31 <= tq) & (c < 255)
        cmpf[:, i, :] = np.where(vis, 0.0, -30000.0)
        cb = tq // 64
        forced = (s == 0) | (s == cb) | (s == cb - 1)
        nsab[:, i, :] = np.where(s > cb, -10000.0, np.where(forced, 10000.0, 0.0))
        cur = (4 * i + j) // 2
        mobab[:, i, :] = np.where(n < cur, 0.0, -30000.0)
        mobav[:, i, :] = np.where(n < cur, 1.0, 0.0)
    cmpb = np.where(cmpf < 0, -1.0, 0.0)
    return dict(
        cosq=cosq, sinq=sinq,
        cmask=cm.reshape(128, -1).astype(bf), wmask=wm.reshape(128, -1).astype(bf),
        cmpf=cmpf.reshape(128, -1), cmpb=cmpb.reshape(128, -1).astype(bf),
        nsab=nsab.reshape(128, -1), mobab=mobab.reshape(128, -1), mobav=mobav.reshape(128, -1),
    )


def make_in_maps(inputs):
    bf = ml_dtypes.bfloat16
    f = lambda a: np.ascontiguousarray(np.asarray(a), dtype=np.float32)
    x = f(inputs["x"])
    c = f(inputs["c"])
    wadaT = np.ascontiguousarray(f(inputs["w_ada"])[0].T)
    bada = np.ascontiguousarray(f(inputs["b_ada"])[0].reshape(96, 128).T)
    norms = np.stack([f(inputs["pre_norm_mix"])[0], f(inputs["post_norm_mix"])[0],
                      f(inputs["pre_norm_ffn"])[0], f(inputs["post_norm_ffn"])[0]], 0)
    normsT = np.ascontiguousarray(norms.reshape(4, 16, 128).transpose(2, 0, 1).reshape(128, 64))
    cosk, sink = _tables()
    w_in = f(inputs["w_in"])[0]
    grp_cols = [FM[2 * g][0] for g in range(8)] + [TM[2 * g] for g in range(6)] + \
               [C_QN + 256 * g for g in range(4)] + [C_QM + 256 * g for g in range(4)]

    def pm(w, ncol):
        return np.ascontiguousarray(w.reshape(-1, 128, ncol).transpose(1, 0, 2).reshape(128, -1))
    w_in_r = np.stack([pm(w_in[:, c0:c0 + 256], 256) for c0 in grp_cols], 0)
    w_g_r = pm(w_in[:, C_G:C_G + 24], 24)
    w_o = f(inputs["w_o"])[0]
    w_o_r = np.stack([pm(w_o[:, cg * 512:(cg + 1) * 512], 512) for cg in range(4)], 0)
    w_up = f(inputs["w_up"])[0]
    w_up_r = np.stack([pm(w_up[:, fc * 128:(fc + 1) * 128], 128) for fc in range(64)], 0)
    w_down = f(inputs["w_down"])[0]
    w_down_r = np.stack([pm(w_down[kg * 1024:(kg + 1) * 1024, cg * 512:(cg + 1) * 512], 512) for cg in range(4) for kg in range(8)], 0)

    def pm1(w):
        return np.ascontiguousarray(w.reshape(32, 128, 128).transpose(1, 0, 2).reshape(128, -1))
    shared = dict(
        wadaT=wadaT, bada=bada, normsT=normsT, w_in_r=w_in_r, w_g_r=w_g_r,
        ckw1=pm1(f(inputs["cmp_k_w1"])[0]), cvw1=pm1(f(inputs["cmp_v_w1"])[0]),
        ckw2=f(inputs["cmp_k_w2"])[0], cvw2=f(inputs["cmp_v_w2"])[0],
        ckposT=np.ascontiguousarray(f(inputs["cmp_k_pos"])[0].T), cvposT=np.ascontiguousarray(f(inputs["cmp_v_pos"])[0].T),
        w_o_r=w_o_r, w_up_r=w_up_r, w_down_r=w_down_r,
        cosk=cosk, sink=sink,
        idb4=(np.tile(np.eye(128, dtype=np.float32), (1, 4)) * NEG).astype(bf),
        idf=np.eye(128, dtype=np.float32),
    )
    ctabs = [_core_tables(j, cosk, sink) for j in range(4)]
    in_maps = []
    for core in range(8):
        b, j = core // 4, core % 4
        m = dict(shared)
        m.update(ctabs[j])
        m["xb"] = x[b]
        m["xo"] = np.ascontiguousarray(x[b].reshape(NL, 4, 128, D)[:, j].reshape(NL * 128, D))
        m["cbc"] = np.ascontiguousarray(np.broadcast_to(c[b][None, :], (128, D)))
        in_maps.append(m)
    return in_maps


_NC_CACHE = {}


def kernel(**inputs):
    if "nc" not in _NC_CACHE:
        _NC_CACHE["nc"] = build_program(debug=False)
    nc = _NC_CACHE["nc"]
    in_maps = make_in_maps(inputs)
    res = run_bass_kernel_spmd(nc, in_maps, core_ids=list(range(8)))
    outp = np.zeros((2, T, D), np.float32)
    for core in range(8):
        b, j = core // 4, core % 4
        outp[b].reshape(NL, 4, 128, D)[:, j] = np.asarray(res.results[core]["out"]).reshape(NL, 128, D)
    return outp
```

```python
import os
import contextlib
import numpy as np
import ml_dtypes
import concourse.bass as bass
import concourse.mybir as mybir
from concourse.bass_utils import run_bass_kernel_spmd

F32 = mybir.dt.float32
BF16 = mybir.dt.bfloat16
AF = mybir.ActivationFunctionType
ALU = mybir.AluOpType
AX = mybir.AxisListType

D = 2048
T = 4096
NT = 32
NL = 8
KC = 16
DFF = 8192
EPS = 1e-6
NEG = 8192.0
ARENA_BYTES = 176 * 1024

C_QN, C_KC, C_VC, C_KS, C_VS, C_KW, C_VW, C_G, C_QM, C_KM, C_VM = (
    0, 1024, 1280, 1536, 1792, 2048, 2304, 2560, 2584, 3608, 4632)
FM = [(C_KC, True), (C_KC + 128, True), (C_VC, False), (C_VC + 128, False),
      (C_KS, True), (C_KS + 128, True), (C_KW, True), (C_KW + 128, True)] + \
     [(C_KM + 128 * h, True) for h in range(8)]
TM = [C_VS, C_VS + 128, C_VW, C_VW + 128] + [C_VM + 128 * h for h in range(8)]


class Buf:
    __slots__ = ("w", "r")

    def __init__(self):
        self.w = None
        self.r = {}


class Prog:
    ENGS = ("pe", "act", "dve", "pool", "sp")

    def __init__(self, nc):
        self.nc = nc
        self.ops = {e: [] for e in self.ENGS}
        self.cnt = {e: 0 for e in self.ENGS}
        self.dcnt = {}

    def _deps(self, eng, reads, writes):
        waits = {}

        def need(s, v):
            if eng == "pe" and s == "pe":
                return
            if waits.get(s, 0) < v:
                waits[s] = v
        for b in reads:
            if b.w is not None:
                need(*b.w)
        for b in writes:
            if b.w is not None:
                need(*b.w)
            for s, v in b.r.items():
                need(s, v)
        return waits

    def _commit(self, tok, reads, writes):
        s, v = tok
        for b in reads:
            if b.r.get(s, 0) < v:
                b.r[s] = v
        for b in writes:
            b.w = tok
            b.r = {}

    def op(self, eng, fn, reads=(), writes=()):
        waits = self._deps(eng, reads, writes)
        self.cnt[eng] += 1
        tok = (eng, self.cnt[eng])
        self._commit(tok, reads, writes)
        self.ops[eng].append((waits, fn, eng, 1))

    def dma(self, q, sem, fn, reads=(), writes=()):
        waits = self._deps(q, reads, writes)
        self.dcnt[sem] = self.dcnt.get(sem, 0) + 16
        tok = (sem, self.dcnt[sem])
        self._commit(tok, reads, writes)
        self.ops[q].append((waits, fn, sem, 16))

    def barrier(self):
        waits = {e: c for e, c in self.cnt.items() if c > 0 and e != "sp"}
        waits.update(self.dcnt)
        for e in self.ENGS:
            w = dict(waits)
            if e == "pe":
                w.pop("pe", None)
            self.ops[e].append((w, None, None, 0))

    def final_wait(self, eng, bufs):
        waits = self._deps(eng, bufs, bufs)
        self.ops[eng].append((waits, None, None, 0))

    def emit(self):
        nc = self.nc
        semnames = list(self.ENGS[:4]) + sorted(self.dcnt.keys())
        with contextlib.ExitStack() as st:
            sems = {n: st.enter_context(nc.semaphore("s_" + n)) for n in semnames}
            block = st.enter_context(nc.Block())
            engmap = {"pe": block.tensor, "act": block.scalar, "dve": block.vector,
                      "pool": block.gpsimd, "sp": block.sync}
            for e in self.ENGS:
                ops = self.ops[e]
                if not ops:
                    continue

                def section(engine, ops=ops):
                    known = {}
                    for waits, fn, semn, inc in ops:
                        for s, v in waits.items():
                            if known.get(s, 0) < v:
                                engine.wait_ge(sems[s], v)
                                known[s] = v
                        if fn is not None:
                            fn(engine).then_inc(sems[semn], inc)
                engmap[e](section)


class TV:
    __slots__ = ("ap", "b")

    def __init__(self, ap):
        self.ap = ap
        self.b = Buf()


class Arena:
    def __init__(self, tens, nbytes):
        self.T = tens
        self.cap = nbytes
        self.top = 0

    def alloc(self, shape, dtype):
        n = int(np.prod(shape[1:]))
        esz = 4 if dtype == F32 else 2
        nb = (n * esz + 63) // 64 * 64
        off = self.top
        self.top += nb
        assert self.top <= self.cap, ("SBUF arena overflow", self.top, self.cap)
        ap = self.T[:, off // 4:(off + nb) // 4]
        if dtype == BF16:
            ap = ap.bitcast(BF16)
        ap = ap[:, 0:n]
        if len(shape) == 3:
            ap = ap.rearrange("p (a b) -> p a b", a=shape[1])
        elif len(shape) == 4:
            ap = ap.rearrange("p (a b c) -> p a b c", a=shape[1], b=shape[2])
        return TV(ap)


def build_program(debug=False, stop_after=None, skip=()):
    nc = bass.Bass("TRN2", target_bir_lowering=False)

    def din(name, shape, dt=F32):
        return nc.dram_tensor(name, list(shape), dt, kind="ExternalInput").ap()

    def dscr(name, shape, dt):
        return nc.dram_tensor(name, list(shape), dt, kind="ExternalOutput" if debug else "Internal").ap()

    xb = din("xb", [T, D])
    xo = din("xo", [NL * 128, D])
    wadaT = din("wadaT", [6 * D, D])
    bada = din("bada", [128, 96])
    cbc = din("cbc", [128, D])
    normsT = din("normsT", [128, 64])
    w_in_r = din("w_in_r", [22, 128, KC * 256])
    w_g_r = din("w_g_r", [128, KC * 24])
    cw1 = [din("ckw1", [128, 32 * 128]), din("cvw1", [128, 32 * 128])]
    cw2 = [din("ckw2", [128, 128]), din("cvw2", [128, 128])]
    cposT = [din("ckposT", [128, 32]), din("cvposT", [128, 32])]
    w_o_r = din("w_o_r", [4, 128, KC * 512])
    w_up_r = din("w_up_r", [64, 128, KC * 128])
    w_down_r = din("w_down_r", [32, 128, 8 * 512])
    cosk = din("cosk", [128, T])
    sink = din("sink", [128, T])
    cosq = din("cosq", [128, NL * 128])
    sinq = din("sinq", [128, NL * 128])
    cmask_d = din("cmask", [128, 4 * 128], BF16)
    wmask_d = din("wmask", [128, 8 * 128], BF16)
    cmpf_d = din("cmpf", [128, NL * 256])
    cmpb_d = din("cmpb", [128, NL * 256], BF16)
    nsab_d = din("nsab", [128, NL * 64])
    mobab_d = din("mobab", [128, NL * 16])
    mobav_d = din("mobav", [128, NL * 16])
    idb4_d = din("idb4", [128, 512], BF16)
    idf_d = din("idf", [128, 128])
    out = nc.dram_tensor("out", [NL * 128, D], F32, kind="ExternalOutput").ap()
    KT = dscr("KT", [16, 128, T], BF16)
    VD = dscr("VD", [NT, 128, 12 * 129], BF16)
    X1S = dscr("X1S", [NL * 128, D], F32)
    FS = dscr("FSC", [NL * 128, D], F32)
    if debug:
        dbg_mod = dscr("dbg_mod", [128, 96], F32)
        dbg_qt = dscr("dbg_qt", [128, 16 * 1024], BF16)
        dbg_g = dscr("dbg_g", [128, NL * 24], F32)
        dbg_ot = dscr("dbg_ot", [128, 16 * 1024], BF16)
        dbg_kc = dscr("dbg_kc", [128, 2 * 256], BF16)
        dbg_vc = dscr("dbg_vc", [128, 4 * 129], BF16)
        dbg_km = dscr("dbg_km", [128, 8 * 16], BF16)

    kt_b = {}
    vd_b = {}
    x1_b = [Buf() for _ in range(NL)]
    fs_b = {}
    out_b = [Buf() for _ in range(NL)]
    dbg_b = [Buf() for _ in range(8)]

    def ktb(idx, tc):
        return kt_b.setdefault((idx, tc), Buf())

    def vdb(gt, grp):
        return vd_b.setdefault((gt, grp), Buf())

    def fsb(i, cg):
        return fs_b.setdefault((i, cg), Buf())

    with contextlib.ExitStack() as st:
        arena_t = st.enter_context(nc.sbuf_tensor("arena", [128, ARENA_BYTES // 4], F32))
        PS = [TV(st.enter_context(nc.psum_tensor("ps%d" % k, [128, 512], F32))[:]) for k in range(8)]
        ar = Arena(arena_t, ARENA_BYTES)
        p = Prog(nc)
        op = p.op

        semmap = {}

        def dma(q, sem, out_ap, in_ap, reads=(), writes=()):
            if sem not in semmap:
                semmap[sem] = "d%02d" % len(semmap)
            p.dma(q, semmap[sem], lambda e: e.dma_start(out=out_ap, in_=in_ap), reads=reads, writes=writes)

        _raw_barrier = p.barrier

        def _barrier():
            _raw_barrier()
            semmap.clear()
        p.barrier = _barrier

        IDF = ar.alloc([128, 128], F32)
        IDB4 = ar.alloc([128, 512], BF16)
        MODT = ar.alloc([128, 96], F32)
        NRM = ar.alloc([128, 4, 16], F32)
        AM = ar.alloc([128, 16], F32)
        AFF = ar.alloc([128, 16], F32)
        AM8 = ar.alloc([128, 16], F32)
        AFF8 = ar.alloc([128, 16], F32)
        GGS = ar.alloc([128, 2, 16], F32)
        SSF = ar.alloc([128, NL, 4], F32)
        NST = [ar.alloc([128, 4], F32) for _ in range(2)]
        dma("sp", "c0", IDF.ap, idf_d, writes=[IDF.b])
        IDB1 = ar.alloc([128, 128], BF16)
        op("dve", lambda e: e.tensor_copy(out=IDB1.ap, in_=IDF.ap), reads=[IDF.b], writes=[IDB1.b])
        dma("sp", "c1", IDB4.ap, idb4_d, writes=[IDB4.b])
        dma("sp", "c2", NRM.ap, normsT.rearrange("p (a b) -> p a b", a=4), writes=[NRM.b])
        persist_mark = ar.top
        SLOT1 = persist_mark
        SLOT2 = persist_mark + 32 * 1024
        PB = persist_mark + 64 * 1024
        ar.top = PB
        G = ar.alloc([128, NL, 24], F32)
        KCT = ar.alloc([128, 2, 256], BF16)
        VCP = ar.alloc([128, 2, 2, 129], BF16)
        KMT = ar.alloc([128, 8, 16], BF16)
        ZR = ar.alloc([128, 258], BF16)
        SLOT3 = ar.top
        ar.top = persist_mark

        def phase_mod():
            SCB = ar.alloc([128, D], F32)
            BAD = ar.alloc([128, 96], F32)
            JNK = ar.alloc([128, D], BF16)
            WT = [ar.alloc([128, D], F32) for _ in range(3)]
            dma("sp", "m0", SCB.ap, cbc, writes=[SCB.b])
            dma("sp", "m1", BAD.ap, bada, writes=[BAD.b])
            op("act", lambda e: e.activation(out=SCB.ap, in_=SCB.ap, func=AF.Silu), reads=[SCB.b], writes=[SCB.b])
            for m in range(96):
                w = WT[m % 3]
                dma("sp", "mw%d" % (m % 3), w.ap, wadaT[m * 128:(m + 1) * 128, :], writes=[w.b])
                op("dve", lambda e, w=w, m=m: e.scalar_tensor_tensor(
                    out=JNK.ap, in0=w.ap, scalar=1.0, in1=SCB.ap, op0=ALU.mult, op1=ALU.mult,
                    accum_out=MODT.ap[:, m:m + 1]), reads=[w.b, SCB.b], writes=[JNK.b, MODT.b])
            op("dve", lambda e: e.tensor_tensor(out=MODT.ap, in0=MODT.ap, in1=BAD.ap, op=ALU.add),
               reads=[MODT.b, BAD.b], writes=[MODT.b])
            op("dve", lambda e: e.scalar_tensor_tensor(out=AM.ap, in0=MODT.ap[:, 16:32], scalar=1.0, in1=NRM.ap[:, 0, :],
                                                        op0=ALU.add, op1=ALU.mult), reads=[MODT.b, NRM.b], writes=[AM.b])
            op("dve", lambda e: e.scalar_tensor_tensor(out=AFF.ap, in0=MODT.ap[:, 64:80], scalar=1.0, in1=NRM.ap[:, 2, :],
                                                        op0=ALU.add, op1=ALU.mult), reads=[MODT.b, NRM.b], writes=[AFF.b])
            op("dve", lambda e: e.tensor_scalar(out=AM8.ap, in0=AM.ap, scalar1=1.0 / NEG, scalar2=None, op0=ALU.mult), reads=[AM.b], writes=[AM8.b])
            op("dve", lambda e: e.tensor_scalar(out=AFF8.ap, in0=AFF.ap, scalar1=1.0 / NEG, scalar2=None, op0=ALU.mult), reads=[AFF.b], writes=[AFF8.b])
            op("dve", lambda e: e.tensor_tensor(out=GGS.ap[:, 0, :], in0=MODT.ap[:, 32:48], in1=NRM.ap[:, 1, :], op=ALU.mult),
               reads=[MODT.b, NRM.b], writes=[GGS.b])
            op("dve", lambda e: e.tensor_tensor(out=GGS.ap[:, 1, :], in0=MODT.ap[:, 80:96], in1=NRM.ap[:, 3, :], op=ALU.mult),
               reads=[MODT.b, NRM.b, GGS.b], writes=[GGS.b])
            if debug:
                dma("sp", "dbg0", dbg_mod, MODT.ap, reads=[MODT.b], writes=[dbg_b[0]])

        def norm_tile_to_hT(SRC, XS, JN, hT, col0, avec, shift_cols, bankset, ti):
            ST = NST[ti % 2]
            op("act", lambda e: e.activation(out=JN.ap, in_=SRC.ap, func=AF.Square, accum_out=ST.ap[:, 0:1]),
               reads=[SRC.b, ST.b], writes=[JN.b, ST.b])
            op("act", lambda e: e.activation(out=ST.ap[:, 1:2], in_=ST.ap[:, 0:1], func=AF.Sqrt, scale=1.0 / D, bias=EPS),
               reads=[ST.b], writes=[ST.b])
            op("dve", lambda e: e.reciprocal(out=ST.ap[:, 2:3], in_=ST.ap[:, 1:2]), reads=[ST.b], writes=[ST.b])
            op("act", lambda e: e.activation(out=XS.ap, in_=SRC.ap, func=AF.Copy, scale=ST.ap[:, 2:3]),
               reads=[SRC.b, XS.b, ST.b], writes=[XS.b])
            for kc in range(KC):
                bk = PS[bankset * 4 + kc // 4]
                sub = bk.ap.bitcast(BF16)[:, (kc % 4) * 128:(kc % 4) * 128 + 128]
                op("pe", lambda e, sub=sub, kc=kc: e.transpose(out=sub, in_=XS.ap[:, kc * 128:(kc + 1) * 128], identity=IDB1.ap),
                   reads=[XS.b, IDB1.b], writes=[bk.b])
            for kc in range(KC):
                bk = PS[bankset * 4 + kc // 4]
                sub = bk.ap.bitcast(BF16)[:, (kc % 4) * 128:(kc % 4) * 128 + 128]
                dst = hT.ap[:, kc, col0:col0 + 128]
                if False:
                    op("act", lambda e, sub=sub, dst=dst, kc=kc: e.activation(
                        out=dst, in_=sub, func=AF.Identity, scale=avec.ap[:, kc:kc + 1], bias=shift_cols[:, kc:kc + 1]),
                        reads=[bk.b, avec.b, MODT.b], writes=[hT.b])
                else:
                    op("dve", lambda e, sub=sub, dst=dst, kc=kc: e.tensor_scalar(
                        out=dst, in0=sub, scalar1=avec.ap[:, kc:kc + 1], scalar2=shift_cols[:, kc:kc + 1],
                        op0=ALU.mult, op1=ALU.add), reads=[bk.b, avec.b, MODT.b], writes=[hT.b])

        def rope_evac(bank, COS, SIN, ccol, dst_ap, dst_b, tset, n=512):
            T1, T2 = tset
            op("dve", lambda e: e.tensor_tensor(out=T1.ap[:, 0:n], in0=bank.ap[:, 0:n], in1=COS.ap[:, ccol:ccol + n], op=ALU.mult),
               reads=[bank.b, COS.b], writes=[T1.b])
            op("dve", lambda e: e.tensor_tensor(out=T2.ap[0:64, 0:n], in0=bank.ap[64:128, 0:n], in1=SIN.ap[64:128, ccol:ccol + n], op=ALU.mult),
               reads=[bank.b, SIN.b], writes=[T2.b])
            op("dve", lambda e: e.tensor_tensor(out=T2.ap[64:128, 0:n], in0=bank.ap[0:64, 0:n], in1=SIN.ap[0:64, ccol:ccol + n], op=ALU.mult),
               reads=[bank.b, SIN.b, T2.b], writes=[T2.b])
            op("dve", lambda e: e.tensor_tensor(out=dst_ap, in0=T1.ap[:, 0:n], in1=T2.ap[:, 0:n], op=ALU.add),
               reads=[T1.b, T2.b], writes=[dst_b])

        def phase_kv():
            HT = ar.alloc([128, KC, T], BF16)
            m1 = ar.top
            XS = [ar.alloc([128, D], F32) for _ in range(4)]
            JN = [ar.alloc([128, D], BF16) for _ in range(2)]
            for gt in range(NT):
                xs = XS[gt % 4]
                dma("sp", "xs%d" % (gt % 4), xs.ap, xb[gt * 128:(gt + 1) * 128, :], writes=[xs.b])
                norm_tile_to_hT(xs, JN[gt % 2], JN[gt % 2], HT, gt * 128, AM, MODT.ap[:, 0:16], gt % 2, gt)
            p.barrier()
            ar.top = m1
            WB = [ar.alloc([128, KC, 256], BF16) for _ in range(2)]
            TS = [(ar.alloc([128, 512], F32), ar.alloc([128, 512], F32)) for _ in range(2)]
            CS = [(ar.alloc([128, 512], F32), ar.alloc([128, 512], F32)) for _ in range(2)]
            KS = [ar.alloc([128, 512], BF16) for _ in range(4)]
            VS = [ar.alloc([128, 2, 129], BF16) for _ in range(4)]
            for v in VS:
                op("pool", lambda e, v=v: e.memset(v.ap, 1.0), writes=[v.b])
            n_ev = 0
            cs_n = 0
            for grp in range(8):
                wb = WB[grp % 2]
                dma("pool", "wb%d" % (grp % 2), wb.ap, w_in_r[grp].rearrange("p (kc c) -> p kc c", kc=KC), writes=[wb.b])
                roped = FM[2 * grp][1]
                for tc in range(8):
                    cs = None
                    if roped:
                        cs = CS[cs_n % 2]
                        dma("sp", "cs%da" % (cs_n % 2), cs[0].ap, cosk[:, tc * 512:(tc + 1) * 512], writes=[cs[0].b])
                        dma("sp", "cs%db" % (cs_n % 2), cs[1].ap, sink[:, tc * 512:(tc + 1) * 512], writes=[cs[1].b])
                        cs_n += 1
                    for cc in range(2):
                        idx = 2 * grp + cc
                        bank = PS[n_ev % 8]
                        for kc in range(KC):
                            op("pe", lambda e, bank=bank, kc=kc, cc=cc, wb=wb, tc=tc: e.matmul(
                                bank.ap, lhsT=wb.ap[:, kc, cc * 128:(cc + 1) * 128], rhs=HT.ap[:, kc, tc * 512:(tc + 1) * 512],
                                start=(kc == 0), stop=(kc == KC - 1)), reads=[wb.b, HT.b], writes=[bank.b])
                        ks = KS[n_ev % 4]
                        if roped:
                            rope_evac(bank, cs[0], cs[1], 0, ks.ap, ks.b, TS[n_ev % 2])
                        else:
                            op("act", lambda e, bank=bank, ks=ks: e.copy(out=ks.ap, in_=bank.ap), reads=[bank.b], writes=[ks.b])
                        dma("sp", "ks%d" % (n_ev % 4), KT[idx, :, tc * 512:(tc + 1) * 512], ks.ap, reads=[ks.b], writes=[ktb(idx, tc)])
                        n_ev += 1
            for grp in range(6):
                wb = WB[grp % 2]
                dma("pool", "wb%d" % (grp % 2), wb.ap, w_in_r[8 + grp].rearrange("p (kc c) -> p kc c", kc=KC), writes=[wb.b])
                for gt in range(NT):
                    bank = PS[n_ev % 8]
                    for kc in range(KC):
                        op("pe", lambda e, bank=bank, kc=kc, wb=wb, gt=gt: e.matmul(
                            bank.ap[:, 0:256], lhsT=HT.ap[:, kc, gt * 128:(gt + 1) * 128], rhs=wb.ap[:, kc, :],
                            start=(kc == 0), stop=(kc == KC - 1)), reads=[wb.b, HT.b], writes=[bank.b])
                    vs = VS[n_ev % 4]
                    src = bank.ap[:, 0:256].rearrange("p (h d) -> p h d", h=2)
                    if n_ev % 2 == 0:
                        op("act", lambda e, src=src, vs=vs: e.copy(out=vs.ap[:, :, 0:128], in_=src), reads=[bank.b], writes=[vs.b])
                    else:
                        op("dve", lambda e, src=src, vs=vs: e.tensor_copy(out=vs.ap[:, :, 0:128], in_=src), reads=[bank.b], writes=[vs.b])
                    dma("sp", "vs%d" % (n_ev % 4), VD[gt, :, grp * 258:(grp + 1) * 258], vs.ap.rearrange("p h d -> p (h d)"),
                        reads=[vs.b], writes=[vdb(gt, grp)])
                    n_ev += 1

        def phase_q(QT):
            HO = ar.alloc([128, KC, NL * 128], BF16)
            XS = [ar.alloc([128, D], F32) for _ in range(2)]
            JN = [ar.alloc([128, D], BF16) for _ in range(2)]
            WB = [ar.alloc([128, KC, 256], BF16) for _ in range(2)]
            WG = ar.alloc([128, KC, 24], BF16)
            TS = [(ar.alloc([128, 512], F32), ar.alloc([128, 512], F32)) for _ in range(2)]
            CQ = ar.alloc([128, NL * 128], F32)
            SQ = ar.alloc([128, NL * 128], F32)
            dma("sp", "q0", CQ.ap, cosq, writes=[CQ.b])
            dma("sp", "q1", SQ.ap, sinq, writes=[SQ.b])
            dma("pool", "q2", WG.ap, w_g_r.rearrange("p (kc c) -> p kc c", kc=KC), writes=[WG.b])
            for i in range(NL):
                xs = XS[i % 2]
                dma("sp", "xs%d" % (i % 2), xs.ap, xo[i * 128:(i + 1) * 128, :], writes=[xs.b])
                norm_tile_to_hT(xs, JN[i % 2], JN[i % 2], HO, i * 128, AM, MODT.ap[:, 0:16], i % 2, i)
            n_ev = 0
            for grp in range(8):
                wb = WB[grp % 2]
                dma("pool", "wb%d" % (grp % 2), wb.ap, w_in_r[14 + grp].rearrange("p (kc c) -> p kc c", kc=KC), writes=[wb.b])
                for half in range(2):
                    for cc in range(2):
                        head = 2 * grp + cc
                        bank = PS[n_ev % 8]
                        for kc in range(KC):
                            op("pe", lambda e, bank=bank, kc=kc, cc=cc, wb=wb, half=half: e.matmul(
                                bank.ap, lhsT=wb.ap[:, kc, cc * 128:(cc + 1) * 128], rhs=HO.ap[:, kc, half * 512:(half + 1) * 512],
                                start=(kc == 0), stop=(kc == KC - 1)), reads=[wb.b, HO.b], writes=[bank.b])
                        rope_evac(bank, CQ, SQ, half * 512, QT.ap[:, head, half * 512:(half + 1) * 512], QT.b, TS[n_ev % 2])
                        n_ev += 1
            for i in range(NL):
                bank = PS[n_ev % 8]
                for kc in range(KC):
                    op("pe", lambda e, bank=bank, kc=kc, i=i: e.matmul(
                        bank.ap[:, 0:24], lhsT=HO.ap[:, kc, i * 128:(i + 1) * 128], rhs=WG.ap[:, kc, :],
                        start=(kc == 0), stop=(kc == KC - 1)), reads=[WG.b, HO.b], writes=[bank.b])
                op("act", lambda e, bank=bank, i=i: e.activation(out=G.ap[:, i, :], in_=bank.ap[:, 0:24], func=AF.Sigmoid),
                   reads=[bank.b, G.b], writes=[G.b])
                n_ev += 1
            if debug:
                dma("sp", "dbg1", dbg_qt, QT.ap.rearrange("p h t -> p (h t)"), reads=[QT.b], writes=[dbg_b[1]])
                dma("sp", "dbg2", dbg_g, G.ap.rearrange("p i c -> p (i c)"), reads=[G.b], writes=[dbg_b[2]])

        def phase_c():
            KL = [ar.alloc([128, T], BF16) for _ in range(2)]
            W1 = ar.alloc([128, 32, 128], BF16)
            W2 = ar.alloc([128, 128], BF16)
            PT_ = ar.alloc([128, 32], BF16)
            PC = ar.alloc([128, 1], F32)
            HS = ar.alloc([128, 256], BF16)
            KM32 = ar.alloc([128, 16], F32)
            op("pool", lambda e: e.memset(HS.ap, 0.0), writes=[HS.b])
            op("pool", lambda e: e.memset(ZR.ap, 0.0), writes=[ZR.b])
            op("pool", lambda e: e.memset(VCP.ap, 1.0), writes=[VCP.b])
            nl = 0
            for typ in range(2):
                dma("pool", "c3", W1.ap, cw1[typ].rearrange("d (l h) -> d l h", l=32), writes=[W1.b])
                dma("pool", "c4", W2.ap, cw2[typ], writes=[W2.b])
                dma("pool", "c5", PT_.ap, cposT[typ], writes=[PT_.b])
                bank = PS[0]
                for l in range(32):
                    op("pe", lambda e, l=l, bank=bank: e.matmul(bank.ap[:, 0:1], lhsT=W1.ap[:, l, :], rhs=PT_.ap[:, l:l + 1],
                                                               start=(l == 0), stop=(l == 31)), reads=[W1.b, PT_.b], writes=[bank.b])
                op("dve", lambda e, bank=bank: e.tensor_copy(out=PC.ap, in_=bank.ap[:, 0:1]), reads=[bank.b], writes=[PC.b])
                for g in range(2):
                    kl = KL[nl % 2]
                    idx = 2 * typ + g
                    dma("sp", "kl%d" % (nl % 2), kl.ap, KT[idx], reads=[ktb(idx, tc) for tc in range(8)], writes=[kl.b])
                    nl += 1
                    kl3 = kl.ap.rearrange("p (n s) -> p n s", s=16)
                    bank = PS[1 + g]
                    for l in range(32):
                        a = l // 16
                        op("pe", lambda e, l=l, a=a, bank=bank, kl3=kl3: e.matmul(
                            bank.ap[:, 0:255], lhsT=W1.ap[:, l, :], rhs=kl3[:, a:a + 255, l % 16],
                            start=(l == 0), stop=(l == 31)), reads=[W1.b, kl.b], writes=[bank.b])
                    op("act", lambda e, bank=bank: e.activation(out=HS.ap[:, 0:255], in_=bank.ap[:, 0:255], func=AF.Silu, bias=PC.ap[:, 0:1]),
                       reads=[bank.b, PC.b], writes=[HS.b])
                    bank2 = PS[3 + g]
                    if typ == 0:
                        op("pe", lambda e, bank2=bank2: e.matmul(bank2.ap[:, 0:256], lhsT=W2.ap, rhs=HS.ap, start=True, stop=True),
                           reads=[W2.b, HS.b], writes=[bank2.b])
                        op("dve", lambda e, bank2=bank2, g=g: e.tensor_copy(out=KCT.ap[:, g, :], in_=bank2.ap[:, 0:256]),
                           reads=[bank2.b, KCT.b], writes=[KCT.b])
                    else:
                        for ct in range(2):
                            op("pe", lambda e, bank2=bank2, ct=ct: e.matmul(bank2.ap[:, ct * 128:(ct + 1) * 128], lhsT=HS.ap[:, ct * 128:(ct + 1) * 128],
                                                                            rhs=W2.ap, start=True, stop=True), reads=[W2.b, HS.b], writes=[bank2.b])
                        op("dve", lambda e, bank2=bank2, g=g: e.tensor_copy(
                            out=VCP.ap[:, g, :, 0:128], in_=bank2.ap[:, 0:256].rearrange("p (c d) -> p c d", c=2)),
                            reads=[bank2.b, VCP.b], writes=[VCP.b])
            for h in range(8):
                kl = KL[nl % 2]
                dma("sp", "kl%d" % (nl % 2), kl.ap, KT[8 + h], reads=[ktb(8 + h, tc) for tc in range(8)], writes=[kl.b])
                nl += 1
                op("dve", lambda e, kl=kl: e.tensor_reduce(out=KM32.ap, in_=kl.ap.rearrange("p (b k) -> p b k", k=256), axis=AX.X, op=ALU.add),
                   reads=[kl.b], writes=[KM32.b])
                op("act", lambda e, h=h: e.activation(out=KMT.ap[:, h, :], in_=KM32.ap, func=AF.Copy, scale=1.0 / 256),
                   reads=[KM32.b, KMT.b], writes=[KMT.b])
            if debug:
                dma("sp", "dbg3", dbg_kc, KCT.ap.rearrange("p g c -> p (g c)"), reads=[KCT.b], writes=[dbg_b[3]])
                dma("sp", "dbg4", dbg_vc, VCP.ap.rearrange("p g c d -> p (g c d)"), reads=[VCP.b], writes=[dbg_b[4]])
                dma("sp", "dbg5", dbg_km, KMT.ap.rearrange("p h n -> p (h n)"), reads=[KMT.b], writes=[dbg_b[5]])

        def phase_a(QT, OT):
            CM = ar.alloc([128, 4, 128], BF16)
            WM = ar.alloc([128, 8, 128], BF16)
            CMPF = [ar.alloc([128, 256], F32) for _ in range(2)]
            CMPB = ar.alloc([128, NL, 256], BF16)
            NSAB = ar.alloc([128, NL, 64], F32)
            MOBAB = ar.alloc([128, NL, 16], F32)
            MOBAV = ar.alloc([128, NL, 16], F32)
            dma("sp", "a0", CM.ap, cmask_d.rearrange("p (a b) -> p a b", a=4), writes=[CM.b])
            dma("sp", "a1", WM.ap, wmask_d.rearrange("p (a b) -> p a b", a=8), writes=[WM.b])
            dma("sp", "a3", CMPB.ap, cmpb_d.rearrange("p (a b) -> p a b", a=NL), writes=[CMPB.b])
            dma("sp", "a4", NSAB.ap, nsab_d.rearrange("p (a b) -> p a b", a=NL), writes=[NSAB.b])
            dma("sp", "a5", MOBAB.ap, mobab_d.rearrange("p (a b) -> p a b", a=NL), writes=[MOBAB.b])
            dma("sp", "a6", MOBAV.ap, mobav_d.rearrange("p (a b) -> p a b", a=NL), writes=[MOBAV.b])
            KA = [ar.alloc([128, T], BF16) for _ in range(4)]
            VA = [ar.alloc([128, 8, 4 * 129], BF16) for _ in range(4)]
            MEXP = ar.alloc([128, T], BF16)
            PTB = [ar.alloc([128, 512], BF16) for _ in range(3)]
            SM = ar.alloc([128, 4, 256], F32)
            PACC = ar.alloc([128, 256], F32)
            IMP = ar.alloc([128, 64], F32)
            SCW = ar.alloc([128, 64], F32)
            MSEL = ar.alloc([128, 64], F32)
            M8 = ar.alloc([128, 16], F32)
            SS = ar.alloc([128, 16], F32)
            OEV = [ar.alloc([128, 4, 129], F32) for _ in range(3)]
            CF = ar.alloc([128, 3, 4], F32)
            OTOK = ar.alloc([128, 4, 128], F32)
            GM = ar.alloc([128, 4, 16], F32)
            MB = ar.alloc([128, 4, 16], F32)
            MBB = ar.alloc([128, 4, 16], BF16)
            st_n = [0]
            oset_n = [0]

            def oset():
                k = oset_n[0] % 2
                oset_n[0] += 1
                return (PS[3 + 2 * k], PS[4 + 2 * k])

            def oreg(os_, h):
                bk = os_[h // 2]
                return bk, bk.ap[:, (h % 2) * 129:(h % 2) * 129 + 129]

            def evac_o(os_, dst):
                for k in range(2):
                    src = os_[k].ap[:, 0:258].rearrange("p (h d) -> p h d", h=2)
                    if k == 0:
                        op("act", lambda e, src=src: e.copy(out=dst.ap[:, 0:2, :], in_=src), reads=[os_[k].b, dst.b], writes=[dst.b])
                    else:
                        op("dve", lambda e, src=src: e.tensor_copy(out=dst.ap[:, 2:4, :], in_=src), reads=[os_[k].b, dst.b], writes=[dst.b])

            def attn_step(kq_pairs, mask_list, v_aps, v_b, os_, first, last):
                bk = PS[st_n[0] % 3]
                pt = PTB[st_n[0] % 3]
                st_n[0] += 1
                for (c0, ncol, kl, qr, rd), masks in zip(kq_pairs, mask_list):
                    outap = bk.ap[:, c0:c0 + ncol]
                    op("pe", lambda e, outap=outap, kl=kl, qr=qr: e.matmul(outap, lhsT=kl, rhs=qr, start=True, stop=False),
                       reads=rd, writes=[bk.b])
                    for mi, (ml, mr, mrd) in enumerate(masks):
                        op("pe", lambda e, outap=outap, ml=ml, mr=mr, lastm=(mi == len(masks) - 1): e.matmul(
                            outap, lhsT=ml, rhs=mr, start=False, stop=lastm), reads=mrd, writes=[bk.b])
                op("act", lambda e, bk=bk, pt=pt: e.activation(out=pt.ap, in_=bk.ap, func=AF.Exp), reads=[bk.b], writes=[pt.b])
                if first:
                    for k in range(2):
                        op("pe", lambda e, k=k: e.matmul(os_[k].ap[:, 0:258], lhsT=ZR.ap[:, 0:128], rhs=ZR.ap, start=True, stop=False),
                           reads=[ZR.b], writes=[os_[k].b])
                for h in range(4):
                    obk, oap = oreg(os_, h)
                    op("pe", lambda e, oap=oap, pt=pt, h=h: e.matmul(oap, lhsT=pt.ap[:, h * 128:(h + 1) * 128], rhs=v_aps[h],
                                                                     start=False, stop=last), reads=[pt.b, v_b], writes=[obk.b])

            def finalize(osrcs, coefs_fn, feat0, i):
                nb = len(osrcs)
                for bi, oe in enumerate(osrcs):
                    op("dve", lambda e, oe=oe, bi=bi: e.tensor_scalar(out=CF.ap[:, bi, :], in0=oe.ap[:, :, 128], scalar1=1e-30, scalar2=None, op0=ALU.max),
                       reads=[oe.b, CF.b], writes=[CF.b])
                    op("dve", lambda e, bi=bi: e.reciprocal(out=CF.ap[:, bi, :], in_=CF.ap[:, bi, :]), reads=[CF.b], writes=[CF.b])
                    gap = coefs_fn(bi)
                    if gap is not None:
                        op("dve", lambda e, bi=bi, gap=gap: e.tensor_tensor(out=CF.ap[:, bi, :], in0=CF.ap[:, bi, :], in1=gap, op=ALU.mult),
                           reads=[CF.b, G.b], writes=[CF.b])
                for h in range(4):
                    op("dve", lambda e, h=h: e.tensor_scalar(out=OTOK.ap[:, h, :], in0=osrcs[0].ap[:, h, 0:128], scalar1=CF.ap[:, 0, h:h + 1],
                                                            scalar2=None, op0=ALU.mult), reads=[osrcs[0].b, CF.b, OTOK.b], writes=[OTOK.b])
                    for bi in range(1, nb):
                        op("dve", lambda e, h=h, bi=bi: e.scalar_tensor_tensor(
                            out=OTOK.ap[:, h, :], in0=osrcs[bi].ap[:, h, 0:128], scalar=CF.ap[:, bi, h:h + 1], in1=OTOK.ap[:, h, :],
                            op0=ALU.mult, op1=ALU.add), reads=[osrcs[bi].b, CF.b, OTOK.b], writes=[OTOK.b])
                bk = PS[7]
                for h in range(4):
                    op("pe", lambda e, h=h, bk=bk: e.transpose(out=bk.ap[:, h * 128:(h + 1) * 128], in_=OTOK.ap[:, h, :], identity=IDF.ap),
                       reads=[OTOK.b, IDF.b], writes=[bk.b])
                op("act", lambda e, bk=bk: e.copy(out=OT.ap[:, feat0:feat0 + 4, i * 128:(i + 1) * 128],
                                                  in_=bk.ap.rearrange("p (h q) -> p h q", h=4)), reads=[bk.b, OT.b], writes=[OT.b])

            for k in range(4):
                dma("sp", "ka%d" % k, KA[k].ap, KT[4 + k], reads=[ktb(4 + k, tc) for tc in range(8)], writes=[KA[k].b])
            for q4 in range(4):
                dma("sp", "va%d" % q4, VA[q4].ap,
                    VD[8 * q4:8 * q4 + 8, :, 0:516].rearrange("t p c -> p t c"),
                    reads=[vdb(gt, grp) for gt in range(8 * q4, 8 * q4 + 8) for grp in range(2)], writes=[VA[q4].b])
            for i in range(NL):
                nk = 4 * i + 4
                cmpf = CMPF[i % 2]
                dma("sp", "cf%d" % (i % 2), cmpf.ap, cmpf_d[:, i * 256:(i + 1) * 256], writes=[cmpf.b])
                for g in range(2):
                    qr4 = QT.ap[:, 4 * g:4 * g + 4, i * 128:(i + 1) * 128]
                    os_ = oset()
                    for h in range(4):
                        bk = os_[h // 2]
                        op("pe", lambda e, bk=bk, h=h, g=g, i=i: e.matmul(
                            bk.ap[:, (h % 2) * 256:(h % 2) * 256 + 256], lhsT=QT.ap[:, 4 * g + h, i * 128:(i + 1) * 128],
                            rhs=KCT.ap[:, g, :], start=True, stop=True), reads=[QT.b, KCT.b], writes=[bk.b])
                    for k in range(2):
                        op("dve", lambda e, k=k, os_=os_, cmpf=cmpf: e.tensor_tensor(
                            out=SM.ap[:, 2 * k:2 * k + 2, :], in0=os_[k].ap.rearrange("p (h c) -> p h c", h=2),
                            in1=cmpf.ap.unsqueeze(1).to_broadcast([128, 2, 256]), op=ALU.add),
                            reads=[os_[k].b, cmpf.b, SM.b], writes=[SM.b])
                    for h in range(4):
                        op("act", lambda e, h=h: e.activation(out=SM.ap[:, h, :], in_=SM.ap[:, h, :], func=AF.Exp, accum_out=SS.ap[:, h:h + 1]),
                           reads=[SM.b, SS.b], writes=[SM.b, SS.b])
                    op("dve", lambda e: e.tensor_scalar(out=SS.ap[:, 4:8], in0=SS.ap[:, 0:4], scalar1=1e-30, scalar2=None, op0=ALU.max),
                       reads=[SS.b], writes=[SS.b])
                    op("dve", lambda e: e.reciprocal(out=SS.ap[:, 8:12], in_=SS.ap[:, 4:8]), reads=[SS.b], writes=[SS.b])
                    op("dve", lambda e: e.tensor_scalar(out=PACC.ap, in0=SM.ap[:, 0, :], scalar1=SS.ap[:, 8:9], scalar2=None, op0=ALU.mult),
                       reads=[SM.b, SS.b], writes=[PACC.b])
                    for h in range(1, 4):
                        op("dve", lambda e, h=h: e.scalar_tensor_tensor(out=PACC.ap, in0=SM.ap[:, h, :], scalar=SS.ap[:, 8 + h:9 + h], in1=PACC.ap,
                                                                        op0=ALU.mult, op1=ALU.add), reads=[SM.b, SS.b, PACC.b], writes=[PACC.b])
                    P3 = PACC.ap.rearrange("p (s r) -> p s r", r=4)
                    op("dve", lambda e, P3=P3: e.tensor_tensor(out=IMP.ap, in0=P3[:, :, 0], in1=P3[:, :, 1], op=ALU.add), reads=[PACC.b], writes=[IMP.b])
                    op("dve", lambda e, P3=P3: e.tensor_tensor(out=IMP.ap, in0=IMP.ap, in1=P3[:, :, 2], op=ALU.add), reads=[PACC.b, IMP.b], writes=[IMP.b])
                    op("dve", lambda e, P3=P3: e.scalar_tensor_tensor(out=IMP.ap, in0=P3[:, :, 3], scalar=0.5, in1=IMP.ap, op0=ALU.mult, op1=ALU.add),
                       reads=[PACC.b, IMP.b], writes=[IMP.b])
                    op("dve", lambda e, P3=P3: e.scalar_tensor_tensor(out=IMP.ap[:, 1:64], in0=P3[:, 0:63, 3], scalar=0.5, in1=IMP.ap[:, 1:64],
                                                                      op0=ALU.mult, op1=ALU.add), reads=[PACC.b, IMP.b], writes=[IMP.b])
                    op("dve", lambda e, i=i: e.tensor_tensor(out=IMP.ap, in0=IMP.ap, in1=NSAB.ap[:, i, :], op=ALU.add),
                       reads=[IMP.b, NSAB.b], writes=[IMP.b])
                    op("dve", lambda e: e.max(out=M8.ap[:, 0:8], in_=IMP.ap), reads=[IMP.b, M8.b], writes=[M8.b])
                    op("dve", lambda e: e.match_replace(out=SCW.ap, in_to_replace=M8.ap[:, 0:8], in_values=IMP.ap, imm_value=-30000.0),
                       reads=[IMP.b, M8.b], writes=[SCW.b])
                    op("dve", lambda e: e.max(out=M8.ap[:, 8:16], in_=SCW.ap), reads=[SCW.b, M8.b], writes=[M8.b])
                    op("dve", lambda e: e.tensor_scalar(out=MSEL.ap, in0=IMP.ap, scalar1=M8.ap[:, 15:16], scalar2=1.0, op0=ALU.is_ge, op1=ALU.subtract),
                       reads=[IMP.b, M8.b], writes=[MSEL.b])
                    op("dve", lambda e, nk=nk: e.tensor_copy(
                        out=MEXP.ap[:, 0:nk * 128].rearrange("p (s k) -> p s k", k=64),
                        in_=MSEL.ap[:, 0:2 * nk].unsqueeze(2).to_broadcast([128, 2 * nk, 64])), reads=[MSEL.b], writes=[MEXP.b])
                    os_ = oset()
                    for ct in range(2):
                        attn_step([(0, 512, KCT.ap[:, g, ct * 128:(ct + 1) * 128], qr4, [KCT.b, QT.b])],
                                  [[(CMPB.ap[:, i, ct * 128:(ct + 1) * 128], IDB4.ap, [CMPB.b, IDB4.b])]],
                                  [VCP.ap[:, g, ct, :]] * 4, VCP.b, os_, ct == 0, ct == 1)
                    evac_o(os_, OEV[0])
                    os_ = oset()
                    for kt in range(nk):
                        masks = [(MEXP.ap[:, kt * 128:(kt + 1) * 128], IDB4.ap, [MEXP.b, IDB4.b])]
                        if kt >= 4 * i:
                            masks.append((CM.ap[:, kt - 4 * i, :], IDB4.ap, [CM.b, IDB4.b]))
                        attn_step([(0, 512, KA[g].ap[:, kt * 128:(kt + 1) * 128], qr4, [KA[g].b, QT.b])], [masks],
                                  [VA[kt // 8].ap[:, kt % 8, g * 129:(g + 1) * 129]] * 4, VA[kt // 8].b, os_, kt == 0, kt == nk - 1)
                    evac_o(os_, OEV[1])
                    os_ = oset()
                    k0 = max(0, 4 * i - 4)
                    for kt in range(k0, nk):
                        masks = [(WM.ap[:, kt - (4 * i - 4), :], IDB4.ap, [WM.b, IDB4.b])]
                        attn_step([(0, 512, KA[2 + g].ap[:, kt * 128:(kt + 1) * 128], qr4, [KA[2 + g].b, QT.b])], [masks],
                                  [VA[kt // 8].ap[:, kt % 8, (2 + g) * 129:(3 + g) * 129]] * 4, VA[kt // 8].b, os_, kt == k0, kt == nk - 1)
                    evac_o(os_, OEV[2])
                    G3 = G.ap[:, i, :].rearrange("p (h b) -> p h b", b=3)
                    finalize(OEV, lambda bi, G3=G3, g=g: G3[:, 4 * g:4 * g + 4, bi], 4 * g, i)
            for r in range(2):
                for k in range(4):
                    dma("sp", "ka%d" % k, KA[k].ap, KT[8 + 4 * r + k], reads=[ktb(8 + 4 * r + k, tc) for tc in range(8)], writes=[KA[k].b])
                for q4 in range(4):
                    dma("sp", "va%d" % q4, VA[q4].ap,
                        VD[8 * q4:8 * q4 + 8, :, (4 + 4 * r) * 129:(8 + 4 * r) * 129].rearrange("t p c -> p t c"),
                        reads=[vdb(gt, grp) for gt in range(8 * q4, 8 * q4 + 8) for grp in range(2 + 2 * r, 4 + 2 * r)], writes=[VA[q4].b])
                for i in range(NL):
                    nk = 4 * i + 4
                    bk7 = PS[7]
                    for h in range(4):
                        op("pe", lambda e, h=h, r=r, i=i: e.matmul(bk7.ap[:, h * 16:(h + 1) * 16], lhsT=QT.ap[:, 8 + 4 * r + h, i * 128:(i + 1) * 128],
                                                                   rhs=KMT.ap[:, 4 * r + h, :], start=True, stop=True), reads=[QT.b, KMT.b], writes=[bk7.b])
                    op("dve", lambda e, i=i: e.tensor_tensor(out=GM.ap, in0=bk7.ap[:, 0:64].rearrange("p (h n) -> p h n", h=4),
                                                             in1=MOBAB.ap[:, i:i + 1, :].to_broadcast([128, 4, 16]), op=ALU.add),
                       reads=[bk7.b, MOBAB.b], writes=[GM.b])
                    for h in range(4):
                        op("dve", lambda e, h=h: e.max(out=M8.ap[:, 0:8], in_=GM.ap[:, h, :]), reads=[GM.b, M8.b], writes=[M8.b])
                        op("dve", lambda e, h=h: e.tensor_scalar(out=MB.ap[:, h, :], in0=GM.ap[:, h, :], scalar1=M8.ap[:, 2:3], scalar2=1.0,
                                                                op0=ALU.is_ge, op1=ALU.subtract), reads=[GM.b, M8.b, MB.b], writes=[MB.b])
                    op("dve", lambda e, i=i: e.tensor_tensor(out=MBB.ap, in0=MB.ap, in1=MOBAV.ap[:, i:i + 1, :].to_broadcast([128, 4, 16]), op=ALU.mult),
                       reads=[MB.b, MOBAV.b], writes=[MBB.b])
                    os_ = oset()
                    for kt in range(nk):
                        pairs = []
                        masks_all = []
                        for h in range(4):
                            pairs.append((h * 128, 128, KA[h].ap[:, kt * 128:(kt + 1) * 128], QT.ap[:, 8 + 4 * r + h, i * 128:(i + 1) * 128], [KA[h].b, QT.b]))
                            ms = [(MBB.ap[:, h, kt // 2:kt // 2 + 1].to_broadcast([128, 128]), IDB4.ap[:, 0:128], [MBB.b, IDB4.b])]
                            if kt >= 4 * i:
                                ms.append((CM.ap[:, kt - 4 * i, :], IDB4.ap[:, 0:128], [CM.b, IDB4.b]))
                            masks_all.append(ms)
                        attn_step(pairs, masks_all, [VA[kt // 8].ap[:, kt % 8, h * 129:(h + 1) * 129] for h in range(4)], VA[kt // 8].b, os_, kt == 0, kt == nk - 1)
                    evac_o(os_, OEV[0])
                    finalize([OEV[0]], lambda bi: None, 8 + 4 * r, i)
            if debug:
                dma("sp", "dbg6", dbg_ot, OT.ap.rearrange("p h t -> p (h t)"), reads=[OT.b], writes=[dbg_b[6]])

        def bcast_vec(src_ap, src_b, dst):
            for c4 in range(4):
                bk = PS[c4]
                for cc in range(4):
                    c = 4 * c4 + cc
                    op("pe", lambda e, bk=bk, cc=cc, c=c: e.matmul(bk.ap[:, cc * 128:(cc + 1) * 128], lhsT=src_ap[:, c:c + 1].to_broadcast([128, 128]),
                                                                   rhs=IDF.ap, start=True, stop=True), reads=[src_b, IDF.b], writes=[bk.b])
                op("dve", lambda e, bk=bk, c4=c4: e.tensor_copy(out=dst.ap[:, c4 * 512:(c4 + 1) * 512], in_=bk.ap), reads=[bk.b, dst.b], writes=[dst.b])

        def rstd_from_ss(ss_ap, dst_ap, b):
            op("act", lambda e: e.activation(out=dst_ap, in_=ss_ap, func=AF.Sqrt, scale=1.0 / D, bias=EPS), reads=[b], writes=[b])
            op("dve", lambda e: e.reciprocal(out=dst_ap, in_=dst_ap), reads=[b], writes=[b])

        def phase_o(OT, H2):
            WO = ar.alloc([128, KC, D], BF16)
            GGM = ar.alloc([128, D], F32)
            XS = [ar.alloc([128, D], F32) for _ in range(2)]
            X1 = ar.alloc([128, D], F32)
            JN = ar.alloc([128, D], BF16)
            ST = [ar.alloc([128, 8], F32) for _ in range(2)]
            for cg in range(4):
                dma("pool", "wo%d" % cg, WO.ap[:, :, cg * 512:(cg + 1) * 512],
                    w_o_r[cg].rearrange("p (kc c) -> p kc c", kc=KC), writes=[WO.b])
            bcast_vec(GGS.ap[:, 0, :], GGS.b, GGM)
            for i in range(NL):
                bs = 4 * (i % 2)
                xs = XS[i % 2]
                x1 = X1
                stt = ST[i % 2]
                dma("sp", "xs%d" % (i % 2), xs.ap, xo[i * 128:(i + 1) * 128, :], writes=[xs.b])
                for cg in range(4):
                    bk = PS[bs + cg]
                    for kc in range(KC):
                        op("pe", lambda e, bk=bk, kc=kc, cg=cg, i=i: e.matmul(bk.ap, lhsT=OT.ap[:, kc, i * 128:(i + 1) * 128],
                                                                             rhs=WO.ap[:, kc, cg * 512:(cg + 1) * 512], start=(kc == 0), stop=(kc == KC - 1)),
                           reads=[OT.b, WO.b], writes=[bk.b])
                    op("act", lambda e, bk=bk, cg=cg, stt=stt: e.activation(out=JN.ap[:, 0:512], in_=bk.ap, func=AF.Square, accum_out=stt.ap[:, cg:cg + 1]),
                       reads=[bk.b, stt.b, JN.b], writes=[JN.b, stt.b])
                op("dve", lambda e, stt=stt: e.tensor_reduce(out=stt.ap[:, 4:5], in_=stt.ap[:, 0:4], axis=AX.X, op=ALU.add), reads=[stt.b], writes=[stt.b])
                rstd_from_ss(stt.ap[:, 4:5], stt.ap[:, 5:6], stt.b)
                for cg in range(4):
                    bk = PS[bs + cg]
                    op("dve", lambda e, bk=bk, cg=cg, stt=stt, x1=x1: e.scalar_tensor_tensor(
                        out=x1.ap[:, cg * 512:(cg + 1) * 512], in0=bk.ap, scalar=stt.ap[:, 5:6], in1=GGM.ap[:, cg * 512:(cg + 1) * 512],
                        op0=ALU.mult, op1=ALU.mult), reads=[bk.b, stt.b, GGM.b, x1.b], writes=[x1.b])
                op("dve", lambda e, x1=x1, xs=xs: e.tensor_tensor(out=x1.ap, in0=x1.ap, in1=xs.ap, op=ALU.add), reads=[x1.b, xs.b], writes=[x1.b])
                dma("sp", "x1s", X1S[i * 128:(i + 1) * 128, :], x1.ap, reads=[x1.b], writes=[x1_b[i]])
                norm_tile_to_hT(x1, JN, JN, H2, i * 128, AFF, MODT.ap[:, 48:64], (i + 1) % 2, i)

        def phase_f(H2):
            UT = ar.alloc([128, 64, NL * 128], BF16)
            WU = [ar.alloc([128, KC, 128], BF16) for _ in range(2)]
            RR = ar.alloc([128, 512], F32)
            n_ev = 0
            for fc in range(64):
                wu = WU[fc % 2]
                dma("pool", "wu%d" % (fc % 2), wu.ap, w_up_r[fc].rearrange("p (kc c) -> p kc c", kc=KC), writes=[wu.b])
                for half in range(2):
                    bk = PS[n_ev % 8]
                    for kc in range(KC):
                        op("pe", lambda e, bk=bk, kc=kc, wu=wu, half=half: e.matmul(bk.ap, lhsT=wu.ap[:, kc, :], rhs=H2.ap[:, kc, half * 512:(half + 1) * 512],
                                                                                  start=(kc == 0), stop=(kc == KC - 1)), reads=[wu.b, H2.b], writes=[bk.b])
                    op("dve", lambda e, bk=bk: e.tensor_scalar(out=RR.ap, in0=bk.ap, scalar1=0.0, scalar2=None, op0=ALU.max), reads=[bk.b, RR.b], writes=[RR.b])
                    op("dve", lambda e, fc=fc, half=half: e.tensor_tensor(out=UT.ap[:, fc, half * 512:(half + 1) * 512], in0=RR.ap, in1=RR.ap, op=ALU.mult),
                       reads=[RR.b, UT.b], writes=[UT.b])
                    n_ev += 1
            p.barrier()
            if stop_after == "f_up":
                return
            ar.top = SLOT1
            WD = [ar.alloc([128, 8, 512], BF16) for _ in range(2)]
            FSB = [ar.alloc([128, 512], F32) for _ in range(2)]
            JN = ar.alloc([128, 512], BF16)
            assert ar.top <= SLOT2
            n_w = 0
            n_f = 0
            for cg in range(4):
                for kg in range(8):
                    wd = WD[n_w % 2]
                    dma("pool", "wd%d" % (n_w % 2), wd.ap,
                        w_down_r[cg * 8 + kg].rearrange("p (kk c) -> p kk c", kk=8), writes=[wd.b])
                    n_w += 1
                    for kk in range(8):
                        kc = kg * 8 + kk
                        for i in range(NL):
                            bk = PS[i]
                            op("pe", lambda e, bk=bk, kc=kc, kk=kk, wd=wd, i=i: e.matmul(bk.ap, lhsT=UT.ap[:, kc, i * 128:(i + 1) * 128], rhs=wd.ap[:, kk, :],
                                                                                       start=(kc == 0), stop=(kc == 63)), reads=[UT.b, wd.b], writes=[bk.b])
                for i in range(NL):
                    bk = PS[i]
                    fsb_ = FSB[n_f % 2]
                    op("act", lambda e, bk=bk, i=i, cg=cg: e.activation(out=JN.ap, in_=bk.ap, func=AF.Square, accum_out=SSF.ap[:, i, cg:cg + 1]),
                       reads=[bk.b, SSF.b, JN.b], writes=[JN.b, SSF.b])
                    op("dve", lambda e, bk=bk, fsb_=fsb_: e.tensor_copy(out=fsb_.ap, in_=bk.ap), reads=[bk.b, fsb_.b, SSF.b], writes=[fsb_.b])
                    dma("sp", "fs%d" % (n_f % 2), FS[i * 128:(i + 1) * 128, cg * 512:(cg + 1) * 512], fsb_.ap, reads=[fsb_.b], writes=[fsb(i, cg)])
                    n_f += 1
            p.barrier()
            if stop_after == "f_down":
                return
            ar.top = SLOT1
            GGF = ar.alloc([128, D], F32)
            FT = [ar.alloc([128, D], F32) for _ in range(2)]
            XT = [ar.alloc([128, D], F32) for _ in range(2)]
            ST = ar.alloc([128, NL, 2], F32)
            bcast_vec(GGS.ap[:, 1, :], GGS.b, GGF)
            for i in range(NL):
                ft = FT[i % 2]
                xt = XT[i % 2]
                dma("sp", "ft%d" % (i % 2), ft.ap, FS[i * 128:(i + 1) * 128, :], reads=[fsb(i, cg) for cg in range(4)], writes=[ft.b])
                dma("sp", "xt%d" % (i % 2), xt.ap, X1S[i * 128:(i + 1) * 128, :], reads=[x1_b[i]], writes=[xt.b])
                op("dve", lambda e, i=i: e.tensor_reduce(out=ST.ap[:, i, 0:1], in_=SSF.ap[:, i, :], axis=AX.X, op=ALU.add), reads=[SSF.b, ST.b], writes=[ST.b])
                rstd_from_ss(ST.ap[:, i, 0:1], ST.ap[:, i, 1:2], ST.b)
                op("dve", lambda e, i=i, ft=ft: e.scalar_tensor_tensor(out=ft.ap, in0=ft.ap, scalar=ST.ap[:, i, 1:2], in1=GGF.ap, op0=ALU.mult, op1=ALU.mult),
                   reads=[ft.b, ST.b, GGF.b], writes=[ft.b])
                op("dve", lambda e, ft=ft, xt=xt: e.tensor_tensor(out=ft.ap, in0=ft.ap, in1=xt.ap, op=ALU.add), reads=[ft.b, xt.b], writes=[ft.b])
                dma("sp", "out%d" % (i % 2), out[i * 128:(i + 1) * 128, :], ft.ap, reads=[ft.b], writes=[out_b[i]])

        def run_all():
            phase_mod()
            p.barrier()
            if stop_after == "mod":
                return
            ar.top = persist_mark
            if "kv" not in skip:
                phase_kv()
            p.barrier()
            if stop_after == "kv":
                return
            ar.top = SLOT1
            QT = ar.alloc([128, 16, NL * 128], BF16)
            assert ar.top == SLOT2
            ar.top = SLOT3
            phase_q(QT)
            p.barrier()
            if stop_after == "q":
                return
            ar.top = SLOT3
            phase_c()
            p.barrier()
            if stop_after == "c":
                return
            ar.top = SLOT2
            OT = ar.alloc([128, 16, NL * 128], BF16)
            assert ar.top == PB
            ar.top = SLOT3
            if "a" not in skip:
                phase_a(QT, OT)
            p.barrier()
            if stop_after == "a":
                return
            ar.top = SLOT1
            H2 = ar.alloc([128, KC, NL * 128], BF16)
            ar.top = PB
            phase_o(OT, H2)
            p.barrier()
            if stop_after == "o":
                return
            ar.top = SLOT2
            phase_f(H2)

        run_all()
        fin = list(out_b)
        if debug:
            fin += dbg_b + list(kt_b.values()) + list(vd_b.values()) + x1_b + list(fs_b.values())
        p.final_wait("sp", fin)
        p.emit()
    return nc


def _tables():
    inv = (10000.0 ** (-np.arange(0, 128, 2, dtype=np.float32) / np.float32(128))).astype(np.float32)
    pos = np.arange(T, dtype=np.float32)
    ang = (pos[:, None] * inv[None, :]).astype(np.float32)
    cos = np.cos(ang).astype(np.float32).T
    sin = np.sin(ang).astype(np.float32).T
    cosk = np.concatenate([cos, cos], 0)
    sink = np.concatenate([sin, -sin], 0)
    return np.ascontiguousarray(cosk), np.ascontiguousarray(sink)


def _core_tables(j, cosk, sink):
    bf = ml_dtypes.bfloat16
    own = np.concatenate([np.arange(128 * (4 * i + j), 128 * (4 * i + j) + 128) for i in range(NL)])
    sc = np.float32(128 ** -0.5)
    cosq = np.ascontiguousarray(cosk[:, own] * sc).astype(np.float32)
    sinq = np.ascontiguousarray(sink[:, own] * sc).astype(np.float32)
    q = np.arange(128)[:, None]
    k = np.arange(128)[None, :]
    cm = np.zeros((128, 4, 128), np.float32)
    for jp in range(4):
        if jp == j:
            cm[:, jp, :] = np.where(k <= q, 0.0, -1.0)
        elif jp > j:
            cm[:, jp, :] = -1.0
    wm = np.zeros((128, 8, 128), np.float32)
    for jj in range(8):
        dist = 128 * (j + 4 - jj) + q - k
        wm[:, jj, :] = np.where((dist >= 0) & (dist < 512), 0.0, -1.0)
    cmpf = np.zeros((128, NL, 256), np.float32)
    nsab = np.zeros((128, NL, 64), np.float32)
    mobab = np.zeros((128, NL, 16), np.float32)
    mobav = np.zeros((128, NL, 16), np.float32)
    c = np.arange(256)[None, :]
    s = np.arange(64)[None, :]
    n = np.arange(16)[None, :]
    for i in range(NL):
        tq = 128 * (4 * i + j) + np.arange(128)[:, None]
        vis = (16 * c + 31 <= tq) & (c < 255)
        cmpf[:, i, :] = np.where(vis, 0.0, -30000.0)
        cb = tq // 64
        forced = (s == 0) | (s == cb) | (s == cb - 1)
        nsab[:, i, :] = np.where(s > cb, -10000.0, np.where(forced, 10000.0, 0.0))
        cur = (4 * i + j) // 2
        mobab[:, i, :] = np.where(n < cur, 0.0, -30000.0)
        mobav[:, i, :] = np.where(n < cur, 1.0, 0.0)
    cmpb = np.where(cmpf < 0, -1.0, 0.0)
    return dict(
        cosq=cosq, sinq=sinq,
        cmask=cm.reshape(128, -1).astype(bf), wmask=wm.reshape(128, -1).astype(bf),
        cmpf=cmpf.reshape(128, -1), cmpb=cmpb.reshape(128, -1).astype(bf),
        nsab=nsab.reshape(128, -1), mobab=mobab.reshape(128, -1), mobav=mobav.reshape(128, -1),
    )


def make_in_maps(inputs):
    bf = ml_dtypes.bfloat16
    f = lambda a: np.ascontiguousarray(np.asarray(a), dtype=np.float32)
    x = f(inputs["x"])
    c = f(inputs["c"])
    wadaT = np.ascontiguousarray(f(inputs["w_ada"])[0].T)
    bada = np.ascontiguousarray(f(inputs["b_ada"])[0].reshape(96, 128).T)
    norms = np.stack([f(inputs["pre_norm_mix"])[0], f(inputs["post_norm_mix"])[0],
                      f(inputs["pre_norm_ffn"])[0], f(inputs["post_norm_ffn"])[0]], 0)
    normsT = np.ascontiguousarray(norms.reshape(4, 16, 128).transpose(2, 0, 1).reshape(128, 64))
    cosk, sink = _tables()
    w_in = f(inputs["w_in"])[0]
    grp_cols = [FM[2 * g][0] for g in range(8)] + [TM[2 * g] for g in range(6)] + \
               [C_QN + 256 * g for g in range(4)] + [C_QM + 256 * g for g in range(4)]

    def pm(w, ncol):
        return np.ascontiguousarray(w.reshape(-1, 128, ncol).transpose(1, 0, 2).reshape(128, -1))
    w_in_r = np.stack([pm(w_in[:, c0:c0 + 256], 256) for c0 in grp_cols], 0)
    w_g_r = pm(w_in[:, C_G:C_G + 24], 24)
    w_o = f(inputs["w_o"])[0]
    w_o_r = np.stack([pm(w_o[:, cg * 512:(cg + 1) * 512], 512) for cg in range(4)], 0)
    w_up = f(inputs["w_up"])[0]
    w_up_r = np.stack([pm(w_up[:, fc * 128:(fc + 1) * 128], 128) for fc in range(64)], 0)
    w_down = f(inputs["w_down"])[0]
    w_down_r = np.stack([pm(w_down[kg * 1024:(kg + 1) * 1024, cg * 512:(cg + 1) * 512], 512) for cg in range(4) for kg in range(8)], 0)

    def pm1(w):
        return np.ascontiguousarray(w.reshape(32, 128, 128).transpose(1, 0, 2).reshape(128, -1))
    shared = dict(
        wadaT=wadaT, bada=bada, normsT=normsT, w_in_r=w_in_r, w_g_r=w_g_r,
        ckw1=pm1(f(inputs["cmp_k_w1"])[0]), cvw1=pm1(f(inputs["cmp_v_w1"])[0]),
        ckw2=f(inputs["cmp_k_w2"])[0], cvw2=f(inputs["cmp_v_w2"])[0],
        ckposT=np.ascontiguousarray(f(inputs["cmp_k_pos"])[0].T), cvposT=np.ascontiguousarray(f(inputs["cmp_v_pos"])[0].T),
        w_o_r=w_o_r, w_up_r=w_up_r, w_down_r=w_down_r,
        cosk=cosk, sink=sink,
        idb4=(np.tile(np.eye(128, dtype=np.float32), (1, 4)) * NEG).astype(bf),
        idf=np.eye(128, dtype=np.float32),
    )
    ctabs = [_core_tables(j, cosk, sink) for j in range(4)]
    in_maps = []
    for core in range(8):
        b, j = core // 4, core % 4
        m = dict(shared)
        m.update(ctabs[j])
        m["xb"] = x[b]
        m["xo"] = np.ascontiguousarray(x[b].reshape(NL, 4, 128, D)[:, j].reshape(NL * 128, D))
        m["cbc"] = np.ascontiguousarray(np.broadcast_to(c[b][None, :], (128, D)))
        in_maps.append(m)
    return in_maps


_NC_CACHE = {}


def kernel(**inputs):
    if "nc" not in _NC_CACHE:
        _NC_CACHE["nc"] = build_program(debug=False)
    nc = _NC_CACHE["nc"]
    in_maps = make_in_maps(inputs)
    res = run_bass_kernel_spmd(nc, in_maps, core_ids=list(range(8)))
    outp = np.zeros((2, T, D), np.float32)
    for core in range(8):
        b, j = core // 4, core % 4
        outp[b].reshape(NL, 4, 128, D)[:, j] = np.asarray(res.results[core]["out"]).reshape(NL, 128, D)
    return outp
```

```python
import os
import contextlib
import numpy as np
import ml_dtypes
import concourse.bass as bass
import concourse.mybir as mybir
from concourse.bass_utils import run_bass_kernel_spmd

F32 = mybir.dt.float32
BF16 = mybir.dt.bfloat16
AF = mybir.ActivationFunctionType
ALU = mybir.AluOpType
AX = mybir.AxisListType

D = 2048
T = 4096
NT = 32
NL = 8
KC = 16
DFF = 8192
EPS = 1e-6
NEG = 8192.0
ARENA_BYTES = 176 * 1024

C_QN, C_KC, C_VC, C_KS, C_VS, C_KW, C_VW, C_G, C_QM, C_KM, C_VM = (
    0, 1024, 1280, 1536, 1792, 2048, 2304, 2560, 2584, 3608, 4632)
FM = [(C_KC, True), (C_KC + 128, True), (C_VC, False), (C_VC + 128, False),
      (C_KS, True), (C_KS + 128, True), (C_KW, True), (C_KW + 128, True)] + \
     [(C_KM + 128 * h, True) for h in range(8)]
TM = [C_VS, C_VS + 128, C_VW, C_VW + 128] + [C_VM + 128 * h for h in range(8)]


class Buf:
    __slots__ = ("w", "r")

    def __init__(self):
        self.w = None
        self.r = {}


class Prog:
    ENGS = ("pe", "act", "dve", "pool", "sp")

    def __init__(self, nc):
        self.nc = nc
        self.ops = {e: [] for e in self.ENGS}
        self.cnt = {e: 0 for e in self.ENGS}
        self.dcnt = {}

    def _deps(self, eng, reads, writes):
        waits = {}

        def need(s, v):
            if eng == "pe" and s == "pe":
                return
            if waits.get(s, 0) < v:
                waits[s] = v
        for b in reads:
            if b.w is not None:
                need(*b.w)
        for b in writes:
            if b.w is not None:
                need(*b.w)
            for s, v in b.r.items():
                need(s, v)
        return waits

    def _commit(self, tok, reads, writes):
        s, v = tok
        for b in reads:
            if b.r.get(s, 0) < v:
                b.r[s] = v
        for b in writes:
            b.w = tok
            b.r = {}

    def op(self, eng, fn, reads=(), writes=()):
        waits = self._deps(eng, reads, writes)
        self.cnt[eng] += 1
        tok = (eng, self.cnt[eng])
        self._commit(tok, reads, writes)
        self.ops[eng].append((waits, fn, eng, 1))

    def dma(self, q, sem, fn, reads=(), writes=()):
        waits = self._deps(q, reads, writes)
        self.dcnt[sem] = self.dcnt.get(sem, 0) + 16
        tok = (sem, self.dcnt[sem])
        self._commit(tok, reads, writes)
        self.ops[q].append((waits, fn, sem, 16))

    def barrier(self):
        waits = {e: c for e, c in self.cnt.items() if c > 0 and e != "sp"}
        waits.update(self.dcnt)
        for e in self.ENGS:
            w = dict(waits)
            if e == "pe":
                w.pop("pe", None)
            self.ops[e].append((w, None, None, 0))

    def final_wait(self, eng, bufs):
        waits = self._deps(eng, bufs, bufs)
        self.ops[eng].append((waits, None, None, 0))

    def emit(self):
        nc = self.nc
        semnames = list(self.ENGS[:4]) + sorted(self.dcnt.keys())
        with contextlib.ExitStack() as st:
            sems = {n: st.enter_context(nc.semaphore("s_" + n)) for n in semnames}
            block = st.enter_context(nc.Block())
            engmap = {"pe": block.tensor, "act": block.scalar, "dve": block.vector,
                      "pool": block.gpsimd, "sp": block.sync}
            for e in self.ENGS:
                ops = self.ops[e]
                if not ops:
                    continue

                def section(engine, ops=ops):
                    known = {}
                    for waits, fn, semn, inc in ops:
                        for s, v in waits.items():
                            if known.get(s, 0) < v:
                                engine.wait_ge(sems[s], v)
                                known[s] = v
                        if fn is not None:
                            fn(engine).then_inc(sems[semn], inc)
                engmap[e](section)


class TV:
    __slots__ = ("ap", "b")

    def __init__(self, ap):
        self.ap = ap
        self.b = Buf()


class Arena:
    def __init__(self, tens, nbytes):
        self.T = tens
        self.cap = nbytes
        self.top = 0

    def alloc(self, shape, dtype):
        n = int(np.prod(shape[1:]))
        esz = 4 if dtype == F32 else 2
        nb = (n * esz + 63) // 64 * 64
        off = self.top
        self.top += nb
        assert self.top <= self.cap, ("SBUF arena overflow", self.top, self.cap)
        ap = self.T[:, off // 4:(off + nb) // 4]
        if dtype == BF16:
            ap = ap.bitcast(BF16)
        ap = ap[:, 0:n]
        if len(shape) == 3:
            ap = ap.rearrange("p (a b) -> p a b", a=shape[1])
        elif len(shape) == 4:
            ap = ap.rearrange("p (a b c) -> p a b c", a=shape[1], b=shape[2])
        return TV(ap)


def build_program(debug=False, stop_after=None, skip=()):
    nc = bass.Bass("TRN2", target_bir_lowering=False)

    def din(name, shape, dt=F32):
        return nc.dram_tensor(name, list(shape), dt, kind="ExternalInput").ap()

    def dscr(name, shape, dt):
        return nc.dram_tensor(name, list(shape), dt, kind="ExternalOutput" if debug else "Internal").ap()

    xb = din("xb", [T, D])
    xo = din("xo", [NL * 128, D])
    wadaT = din("wadaT", [6 * D, D])
    bada = din("bada", [128, 96])
    cbc = din("cbc", [128, D])
    normsT = din("normsT", [128, 64])
    w_in_r = din("w_in_r", [22, 128, KC * 256])
    w_g_r = din("w_g_r", [128, KC * 24])
    cw1 = [din("ckw1", [128, 32 * 128]), din("cvw1", [128, 32 * 128])]
    cw2 = [din("ckw2", [128, 128]), din("cvw2", [128, 128])]
    cposT = [din("ckposT", [128, 32]), din("cvposT", [128, 32])]
    w_o_r = din("w_o_r", [4, 128, KC * 512])
    w_up_r = din("w_up_r", [64, 128, KC * 128])
    w_down_r = din("w_down_r", [32, 128, 8 * 512])
    cosk = din("cosk", [128, T])
    sink = din("sink", [128, T])
    cosq = din("cosq", [128, NL * 128])
    sinq = din("sinq", [128, NL * 128])
    cmask_d = din("cmask", [128, 4 * 128], BF16)
    wmask_d = din("wmask", [128, 8 * 128], BF16)
    cmpf_d = din("cmpf", [128, NL * 256])
    cmpb_d = din("cmpb", [128, NL * 256], BF16)
    nsab_d = din("nsab", [128, NL * 64])
    mobab_d = din("mobab", [128, NL * 16])
    mobav_d = din("mobav", [128, NL * 16])
    idb4_d = din("idb4", [128, 512], BF16)
    idf_d = din("idf", [128, 128])
    out = nc.dram_tensor("out", [NL * 128, D], F32, kind="ExternalOutput").ap()
    KT = dscr("KT", [16, 128, T], BF16)
    VD = dscr("VD", [NT, 128, 12 * 129], BF16)
    X1S = dscr("X1S", [NL * 128, D], F32)
    FS = dscr("FSC", [NL * 128, D], F32)
    if debug:
        dbg_mod = dscr("dbg_mod", [128, 96], F32)
        dbg_qt = dscr("dbg_qt", [128, 16 * 1024], BF16)
        dbg_g = dscr("dbg_g", [128, NL * 24], F32)
        dbg_ot = dscr("dbg_ot", [128, 16 * 1024], BF16)
        dbg_kc = dscr("dbg_kc", [128, 2 * 256], BF16)
        dbg_vc = dscr("dbg_vc", [128, 4 * 129], BF16)
        dbg_km = dscr("dbg_km", [128, 8 * 16], BF16)

    kt_b = {}
    vd_b = {}
    x1_b = [Buf() for _ in range(NL)]
    fs_b = {}
    out_b = [Buf() for _ in range(NL)]
    dbg_b = [Buf() for _ in range(8)]

    def ktb(idx, tc):
        return kt_b.setdefault((idx, tc), Buf())

    def vdb(gt, grp):
        return vd_b.setdefault((gt, grp), Buf())

    def fsb(i, cg):
        return fs_b.setdefault((i, cg), Buf())

    with contextlib.ExitStack() as st:
        arena_t = st.enter_context(nc.sbuf_tensor("arena", [128, ARENA_BYTES // 4], F32))
        PS = [TV(st.enter_context(nc.psum_tensor("ps%d" % k, [128, 512], F32))[:]) for k in range(8)]
        ar = Arena(arena_t, ARENA_BYTES)
        p = Prog(nc)
        op = p.op

        semmap = {}

        def dma(q, sem, out_ap, in_ap, reads=(), writes=()):
            if sem not in semmap:
                semmap[sem] = "d%02d" % len(semmap)
            p.dma(q, semmap[sem], lambda e: e.dma_start(out=out_ap, in_=in_ap), reads=reads, writes=writes)

        _raw_barrier = p.barrier

        def _barrier():
            _raw_barrier()
            semmap.clear()
        p.barrier = _barrier

        IDF = ar.alloc([128, 128], F32)
        IDB4 = ar.alloc([128, 512], BF16)
        MODT = ar.alloc([128, 96], F32)
        NRM = ar.alloc([128, 4, 16], F32)
        AM = ar.alloc([128, 16], F32)
        AFF = ar.alloc([128, 16], F32)
        AM8 = ar.alloc([128, 16], F32)
        AFF8 = ar.alloc([128, 16], F32)
        GGS = ar.alloc([128, 2, 16], F32)
        SSF = ar.alloc([128, NL, 4], F32)
        NST = [ar.alloc([128, 4], F32) for _ in range(2)]
        dma("sp", "c0", IDF.ap, idf_d, writes=[IDF.b])
        IDB1 = ar.alloc([128, 128], BF16)
        op("dve", lambda e: e.tensor_copy(out=IDB1.ap, in_=IDF.ap), reads=[IDF.b], writes=[IDB1.b])
        dma("sp", "c1", IDB4.ap, idb4_d, writes=[IDB4.b])
        dma("sp", "c2", NRM.ap, normsT.rearrange("p (a b) -> p a b", a=4), writes=[NRM.b])
        persist_mark = ar.top
        SLOT1 = persist_mark
        SLOT2 = persist_mark + 32 * 1024
        PB = persist_mark + 64 * 1024
        ar.top = PB
        G = ar.alloc([128, NL, 24], F32)
        KCT = ar.alloc([128, 2, 256], BF16)
        VCP = ar.alloc([128, 2, 2, 129], BF16)
        KMT = ar.alloc([128, 8, 16], BF16)
        ZR = ar.alloc([128, 258], BF16)
        SLOT3 = ar.top
        ar.top = persist_mark

        def phase_mod():
            SCB = ar.alloc([128, D], F32)
            BAD = ar.alloc([128, 96], F32)
            JNK = ar.alloc([128, D], BF16)
            WT = [ar.alloc([128, D], F32) for _ in range(3)]
            dma("sp", "m0", SCB.ap, cbc, writes=[SCB.b])
            dma("sp", "m1", BAD.ap, bada, writes=[BAD.b])
            op("act", lambda e: e.activation(out=SCB.ap, in_=SCB.ap, func=AF.Silu), reads=[SCB.b], writes=[SCB.b])
            for m in range(96):
                w = WT[m % 3]
                dma("sp", "mw%d" % (m % 3), w.ap, wadaT[m * 128:(m + 1) * 128, :], writes=[w.b])
                op("dve", lambda e, w=w, m=m: e.scalar_tensor_tensor(
                    out=JNK.ap, in0=w.ap, scalar=1.0, in1=SCB.ap, op0=ALU.mult, op1=ALU.mult,
                    accum_out=MODT.ap[:, m:m + 1]), reads=[w.b, SCB.b], writes=[JNK.b, MODT.b])
            op("dve", lambda e: e.tensor_tensor(out=MODT.ap, in0=MODT.ap, in1=BAD.ap, op=ALU.add),
               reads=[MODT.b, BAD.b], writes=[MODT.b])
            op("dve", lambda e: e.scalar_tensor_tensor(out=AM.ap, in0=MODT.ap[:, 16:32], scalar=1.0, in1=NRM.ap[:, 0, :],
                                                        op0=ALU.add, op1=ALU.mult), reads=[MODT.b, NRM.b], writes=[AM.b])
            op("dve", lambda e: e.scalar_tensor_tensor(out=AFF.ap, in0=MODT.ap[:, 64:80], scalar=1.0, in1=NRM.ap[:, 2, :],
                                                        op0=ALU.add, op1=ALU.mult), reads=[MODT.b, NRM.b], writes=[AFF.b])
            op("dve", lambda e: e.tensor_scalar(out=AM8.ap, in0=AM.ap, scalar1=1.0 / NEG, scalar2=None, op0=ALU.mult), reads=[AM.b], writes=[AM8.b])
            op("dve", lambda e: e.tensor_scalar(out=AFF8.ap, in0=AFF.ap, scalar1=1.0 / NEG, scalar2=None, op0=ALU.mult), reads=[AFF.b], writes=[AFF8.b])
            op("dve", lambda e: e.tensor_tensor(out=GGS.ap[:, 0, :], in0=MODT.ap[:, 32:48], in1=NRM.ap[:, 1, :], op=ALU.mult),
               reads=[MODT.b, NRM.b], writes=[GGS.b])
            op("dve", lambda e: e.tensor_tensor(out=GGS.ap[:, 1, :], in0=MODT.ap[:, 80:96], in1=NRM.ap[:, 3, :], op=ALU.mult),
               reads=[MODT.b, NRM.b, GGS.b], writes=[GGS.b])
            if debug:
                dma("sp", "dbg0", dbg_mod, MODT.ap, reads=[MODT.b], writes=[dbg_b[0]])

        def norm_tile_to_hT(SRC, XS, JN, hT, col0, avec, shift_cols, bankset, ti):
            ST = NST[ti % 2]
            op("act", lambda e: e.activation(out=JN.ap, in_=SRC.ap, func=AF.Square, accum_out=ST.ap[:, 0:1]),
               reads=[SRC.b, ST.b], writes=[JN.b, ST.b])
            op("act", lambda e: e.activation(out=ST.ap[:, 1:2], in_=ST.ap[:, 0:1], func=AF.Sqrt, scale=1.0 / D, bias=EPS),
               reads=[ST.b], writes=[ST.b])
            op("dve", lambda e: e.reciprocal(out=ST.ap[:, 2:3], in_=ST.ap[:, 1:2]), reads=[ST.b], writes=[ST.b])
            op("act", lambda e: e.activation(out=XS.ap, in_=SRC.ap, func=AF.Copy, scale=ST.ap[:, 2:3]),
               reads=[SRC.b, XS.b, ST.b], writes=[XS.b])
            for kc in range(KC):
                bk = PS[bankset * 4 + kc // 4]
                sub = bk.ap.bitcast(BF16)[:, (kc % 4) * 128:(kc % 4) * 128 + 128]
                op("pe", lambda e, sub=sub, kc=kc: e.transpose(out=sub, in_=XS.ap[:, kc * 128:(kc + 1) * 128], identity=IDB1.ap),
                   reads=[XS.b, IDB1.b], writes=[bk.b])
            for kc in range(KC):
                bk = PS[bankset * 4 + kc // 4]
                sub = bk.ap.bitcast(BF16)[:, (kc % 4) * 128:(kc % 4) * 128 + 128]
                dst = hT.ap[:, kc, col0:col0 + 128]
                if False:
                    op("act", lambda e, sub=sub, dst=dst, kc=kc: e.activation(
                        out=dst, in_=sub, func=AF.Identity, scale=avec.ap[:, kc:kc + 1], bias=shift_cols[:, kc:kc + 1]),
                        reads=[bk.b, avec.b, MODT.b], writes=[hT.b])
                else:
                    op("dve", lambda e, sub=sub, dst=dst, kc=kc: e.tensor_scalar(
                        out=dst, in0=sub, scalar1=avec.ap[:, kc:kc + 1], scalar2=shift_cols[:, kc:kc + 1],
                        op0=ALU.mult, op1=ALU.add), reads=[bk.b, avec.b, MODT.b], writes=[hT.b])

        def rope_evac(bank, COS, SIN, ccol, dst_ap, dst_b, tset, n=512):
            T1, T2 = tset
            op("dve", lambda e: e.tensor_tensor(out=T1.ap[:, 0:n], in0=bank.ap[:, 0:n], in1=COS.ap[:, ccol:ccol + n], op=ALU.mult),
               reads=[bank.b, COS.b], writes=[T1.b])
            op("dve", lambda e: e.tensor_tensor(out=T2.ap[0:64, 0:n], in0=bank.ap[64:128, 0:n], in1=SIN.ap[64:128, ccol:ccol + n], op=ALU.mult),
               reads=[bank.b, SIN.b], writes=[T2.b])
            op("dve", lambda e: e.tensor_tensor(out=T2.ap[64:128, 0:n], in0=bank.ap[0:64, 0:n], in1=SIN.ap[0:64, ccol:ccol + n], op=ALU.mult),
               reads=[bank.b, SIN.b, T2.b], writes=[T2.b])
            op("dve", lambda e: e.tensor_tensor(out=dst_ap, in0=T1.ap[:, 0:n], in1=T2.ap[:, 0:n], op=ALU.add),
               reads=[T1.b, T2.b], writes=[dst_b])

        def phase_kv():
            HT = ar.alloc([128, KC, T], BF16)
            m1 = ar.top
            XS = [ar.alloc([128, D], F32) for _ in range(4)]
            JN = [ar.alloc([128, D], BF16) for _ in range(2)]
            for gt in range(NT):
                xs = XS[gt % 4]
                dma("sp", "xs%d" % (gt % 4), xs.ap, xb[gt * 128:(gt + 1) * 128, :], writes=[xs.b])
                norm_tile_to_hT(xs, JN[gt % 2], JN[gt % 2], HT, gt * 128, AM, MODT.ap[:, 0:16], gt % 2, gt)
            p.barrier()
            ar.top = m1
            WB = [ar.alloc([128, KC, 256], BF16) for _ in range(2)]
            TS = [(ar.alloc([128, 512], F32), ar.alloc([128, 512], F32)) for _ in range(2)]
            CS = [(ar.alloc([128, 512], F32), ar.alloc([128, 512], F32)) for _ in range(2)]
            KS = [ar.alloc([128, 512], BF16) for _ in range(4)]
            VS = [ar.alloc([128, 2, 129], BF16) for _ in range(4)]
            for v in VS:
                op("pool", lambda e, v=v: e.memset(v.ap, 1.0), writes=[v.b])
            n_ev = 0
            cs_n = 0
            for grp in range(8):
                wb = WB[grp % 2]
                dma("pool", "wb%d" % (grp % 2), wb.ap, w_in_r[grp].rearrange("p (kc c) -> p kc c", kc=KC), writes=[wb.b])
                roped = FM[2 * grp][1]
                for tc in range(8):
                    cs = None
                    if roped:
                        cs = CS[cs_n % 2]
                        dma("sp", "cs%da" % (cs_n % 2), cs[0].ap, cosk[:, tc * 512:(tc + 1) * 512], writes=[cs[0].b])
                        dma("sp", "cs%db" % (cs_n % 2), cs[1].ap, sink[:, tc * 512:(tc + 1) * 512], writes=[cs[1].b])
                        cs_n += 1
                    for cc in range(2):
                        idx = 2 * grp + cc
                        bank = PS[n_ev % 8]
                        for kc in range(KC):
                            op("pe", lambda e, bank=bank, kc=kc, cc=cc, wb=wb, tc=tc: e.matmul(
                                bank.ap, lhsT=wb.ap[:, kc, cc * 128:(cc + 1) * 128], rhs=HT.ap[:, kc, tc * 512:(tc + 1) * 512],
                                start=(kc == 0), stop=(kc == KC - 1)), reads=[wb.b, HT.b], writes=[bank.b])
                        ks = KS[n_ev % 4]
                        if roped:
                            rope_evac(bank, cs[0], cs[1], 0, ks.ap, ks.b, TS[n_ev % 2])
                        else:
                            op("act", lambda e, bank=bank, ks=ks: e.copy(out=ks.ap, in_=bank.ap), reads=[bank.b], writes=[ks.b])
                        dma("sp", "ks%d" % (n_ev % 4), KT[idx, :, tc * 512:(tc + 1) * 512], ks.ap, reads=[ks.b], writes=[ktb(idx, tc)])
                        n_ev += 1
            for grp in range(6):
                wb = WB[grp % 2]
                dma("pool", "wb%d" % (grp % 2), wb.ap, w_in_r[8 + grp].rearrange("p (kc c) -> p kc c", kc=KC), writes=[wb.b])
                for gt in range(NT):
                    bank = PS[n_ev % 8]
                    for kc in range(KC):
                        op("pe", lambda e, bank=bank, kc=kc, wb=wb, gt=gt: e.matmul(
                            bank.ap[:, 0:256], lhsT=HT.ap[:, kc, gt * 128:(gt + 1) * 128], rhs=wb.ap[:, kc, :],
                            start=(kc == 0), stop=(kc == KC - 1)), reads=[wb.b, HT.b], writes=[bank.b])
                    vs = VS[n_ev % 4]
                    src = bank.ap[:, 0:256].rearrange("p (h d) -> p h d", h=2)
                    if n_ev % 2 == 0:
                        op("act", lambda e, src=src, vs=vs: e.copy(out=vs.ap[:, :, 0:128], in_=src), reads=[bank.b], writes=[vs.b])
                    else:
                        op("dve", lambda e, src=src, vs=vs: e.tensor_copy(out=vs.ap[:, :, 0:128], in_=src), reads=[bank.b], writes=[vs.b])
                    dma("sp", "vs%d" % (n_ev % 4), VD[gt, :, grp * 258:(grp + 1) * 258], vs.ap.rearrange("p h d -> p (h d)"),
                        reads=[vs.b], writes=[vdb(gt, grp)])
                    n_ev += 1

        def phase_q(QT):
            HO = ar.alloc([128, KC, NL * 128], BF16)
            XS = [ar.alloc([128, D], F32) for _ in range(2)]
            JN = [ar.alloc([128, D], BF16) for _ in range(2)]
            WB = [ar.alloc([128, KC, 256], BF16) for _ in range(2)]
            WG = ar.alloc([128, KC, 24], BF16)
            TS = [(ar.alloc([128, 512], F32), ar.alloc([128, 512], F32)) for _ in range(2)]
            CQ = ar.alloc([128, NL * 128], F32)
            SQ = ar.alloc([128, NL * 128], F32)
            dma("sp", "q0", CQ.ap, cosq, writes=[CQ.b])
            dma("sp", "q1", SQ.ap, sinq, writes=[SQ.b])
            dma("pool", "q2", WG.ap, w_g_r.rearrange("p (kc c) -> p kc c", kc=KC), writes=[WG.b])
            for i in range(NL):
                xs = XS[i % 2]
                dma("sp", "xs%d" % (i % 2), xs.ap, xo[i * 128:(i + 1) * 128, :], writes=[xs.b])
                norm_tile_to_hT(xs, JN[i % 2], JN[i % 2], HO, i * 128, AM, MODT.ap[:, 0:16], i % 2, i)
            n_ev = 0
            for grp in range(8):
                wb = WB[grp % 2]
                dma("pool", "wb%d" % (grp % 2), wb.ap, w_in_r[14 + grp].rearrange("p (kc c) -> p kc c", kc=KC), writes=[wb.b])
                for half in range(2):
                    for cc in range(2):
                        head = 2 * grp + cc
                        bank = PS[n_ev % 8]
                        for kc in range(KC):
                            op("pe", lambda e, bank=bank, kc=kc, cc=cc, wb=wb, half=half: e.matmul(
                                bank.ap, lhsT=wb.ap[:, kc, cc * 128:(cc + 1) * 128], rhs=HO.ap[:, kc, half * 512:(half + 1) * 512],
                                start=(kc == 0), stop=(kc == KC - 1)), reads=[wb.b, HO.b], writes=[bank.b])
                        rope_evac(bank, CQ, SQ, half * 512, QT.ap[:, head, half * 512:(half + 1) * 512], QT.b, TS[n_ev % 2])
                        n_ev += 1
            for i in range(NL):
                bank = PS[n_ev % 8]
                for kc in range(KC):
                    op("pe", lambda e, bank=bank, kc=kc, i=i: e.matmul(
                        bank.ap[:, 0:24], lhsT=HO.ap[:, kc, i * 128:(i + 1) * 128], rhs=WG.ap[:, kc, :],
                        start=(kc == 0), stop=(kc == KC - 1)), reads=[WG.b, HO.b], writes=[bank.b])
                op("act", lambda e, bank=bank, i=i: e.activation(out=G.ap[:, i, :], in_=bank.ap[:, 0:24], func=AF.Sigmoid),
                   reads=[bank.b, G.b], writes=[G.b])
                n_ev += 1
            if debug:
                dma("sp", "dbg1", dbg_qt, QT.ap.rearrange("p h t -> p (h t)"), reads=[QT.b], writes=[dbg_b[1]])
                dma("sp", "dbg2", dbg_g, G.ap.rearrange("p i c -> p (i c)"), reads=[G.b], writes=[dbg_b[2]])

        def phase_c():
            KL = [ar.alloc([128, T], BF16) for _ in range(2)]
            W1 = ar.alloc([128, 32, 128], BF16)
            W2 = ar.alloc([128, 128], BF16)
            PT_ = ar.alloc([128, 32], BF16)
            PC = ar.alloc([128, 1], F32)
            HS = ar.alloc([128, 256], BF16)
            KM32 = ar.alloc([128, 16], F32)
            op("pool", lambda e: e.memset(HS.ap, 0.0), writes=[HS.b])
            op("pool", lambda e: e.memset(ZR.ap, 0.0), writes=[ZR.b])
            op("pool", lambda e: e.memset(VCP.ap, 1.0), writes=[VCP.b])
            nl = 0
            for typ in range(2):
                dma("pool", "c3", W1.ap, cw1[typ].rearrange("d (l h) -> d l h", l=32), writes=[W1.b])
                dma("pool", "c4", W2.ap, cw2[typ], writes=[W2.b])
                dma("pool", "c5", PT_.ap, cposT[typ], writes=[PT_.b])
                bank = PS[0]
                for l in range(32):
                    op("pe", lambda e, l=l, bank=bank: e.matmul(bank.ap[:, 0:1], lhsT=W1.ap[:, l, :], rhs=PT_.ap[:, l:l + 1],
                                                               start=(l == 0), stop=(l == 31)), reads=[W1.b, PT_.b], writes=[bank.b])
                op("dve", lambda e, bank=bank: e.tensor_copy(out=PC.ap, in_=bank.ap[:, 0:1]), reads=[bank.b], writes=[PC.b])
                for g in range(2):
                    kl = KL[nl % 2]
                    idx = 2 * typ + g
                    dma("sp", "kl%d" % (nl % 2), kl.ap, KT[idx], reads=[ktb(idx, tc) for tc in range(8)], writes=[kl.b])
                    nl += 1
                    kl3 = kl.ap.rearrange("p (n s) -> p n s", s=16)
                    bank = PS[1 + g]
                    for l in range(32):
                        a = l // 16
                        op("pe", lambda e, l=l, a=a, bank=bank, kl3=kl3: e.matmul(
                            bank.ap[:, 0:255], lhsT=W1.ap[:, l, :], rhs=kl3[:, a:a + 255, l % 16],
                            start=(l == 0), stop=(l == 31)), reads=[W1.b, kl.b], writes=[bank.b])
                    op("act", lambda e, bank=bank: e.activation(out=HS.ap[:, 0:255], in_=bank.ap[:, 0:255], func=AF.Silu, bias=PC.ap[:, 0:1]),
                       reads=[bank.b, PC.b], writes=[HS.b])
                    bank2 = PS[3 + g]
                    if typ == 0:
                        op("pe", lambda e, bank2=bank2: e.matmul(bank2.ap[:, 0:256], lhsT=W2.ap, rhs=HS.ap, start=True, stop=True),
                           reads=[W2.b, HS.b], writes=[bank2.b])
                        op("dve", lambda e, bank2=bank2, g=g: e.tensor_copy(out=KCT.ap[:, g, :], in_=bank2.ap[:, 0:256]),
                           reads=[bank2.b, KCT.b], writes=[KCT.b])
                    else:
                        for ct in range(2):
                            op("pe", lambda e, bank2=bank2, ct=ct: e.matmul(bank2.ap[:, ct * 128:(ct + 1) * 128], lhsT=HS.ap[:, ct * 128:(ct + 1) * 128],
                                                                            rhs=W2.ap, start=True, stop=True), reads=[W2.b, HS.b], writes=[bank2.b])
                        op("dve", lambda e, bank2=bank2, g=g: e.tensor_copy(
                            out=VCP.ap[:, g, :, 0:128], in_=bank2.ap[:, 0:256].rearrange("p (c d) -> p c d", c=2)),
                            reads=[bank2.b, VCP.b], writes=[VCP.b])
            for h in range(8):
                kl = KL[nl % 2]
                dma("sp", "kl%d" % (nl % 2), kl.ap, KT[8 + h], reads=[ktb(8 + h, tc) for tc in range(8)], writes=[kl.b])
                nl += 1
                op("dve", lambda e, kl=kl: e.tensor_reduce(out=KM32.ap, in_=kl.ap.rearrange("p (b k) -> p b k", k=256), axis=AX.X, op=ALU.add),
                   reads=[kl.b], writes=[KM32.b])
                op("act", lambda e, h=h: e.activation(out=KMT.ap[:, h, :], in_=KM32.ap, func=AF.Copy, scale=1.0 / 256),
                   reads=[KM32.b, KMT.b], writes=[KMT.b])
            if debug:
                dma("sp", "dbg3", dbg_kc, KCT.ap.rearrange("p g c -> p (g c)"), reads=[KCT.b], writes=[dbg_b[3]])
                dma("sp", "dbg4", dbg_vc, VCP.ap.rearrange("p g c d -> p (g c d)"), reads=[VCP.b], writes=[dbg_b[4]])
                dma("sp", "dbg5", dbg_km, KMT.ap.rearrange("p h n -> p (h n)"), reads=[KMT.b], writes=[dbg_b[5]])

        def phase_a(QT, OT):
            CM = ar.alloc([128, 4, 128], BF16)
            WM = ar.alloc([128, 8, 128], BF16)
            CMPF = [ar.alloc([128, 256], F32) for _ in range(2)]
            CMPB = ar.alloc([128, NL, 256], BF16)
            NSAB = ar.alloc([128, NL, 64], F32)
            MOBAB = ar.alloc([128, NL, 16], F32)
            MOBAV = ar.alloc([128, NL, 16], F32)
            dma("sp", "a0", CM.ap, cmask_d.rearrange("p (a b) -> p a b", a=4), writes=[CM.b])
            dma("sp", "a1", WM.ap, wmask_d.rearrange("p (a b) -> p a b", a=8), writes=[WM.b])
            dma("sp", "a3", CMPB.ap, cmpb_d.rearrange("p (a b) -> p a b", a=NL), writes=[CMPB.b])
            dma("sp", "a4", NSAB.ap, nsab_d.rearrange("p (a b) -> p a b", a=NL), writes=[NSAB.b])
            dma("sp", "a5", MOBAB.ap, mobab_d.rearrange("p (a b) -> p a b", a=NL), writes=[MOBAB.b])
            dma("sp", "a6", MOBAV.ap, mobav_d.rearrange("p (a b) -> p a b", a=NL), writes=[MOBAV.b])
            KA = [ar.alloc([128, T], BF16) for _ in range(4)]
            VA = [ar.alloc([128, 8, 4 * 129], BF16) for _ in range(4)]
            MEXP = ar.alloc([128, T], BF16)
            PTB = [ar.alloc([128, 512], BF16) for _ in range(3)]
            SM = ar.alloc([128, 4, 256], F32)
            PACC = ar.alloc([128, 256], F32)
            IMP = ar.alloc([128, 64], F32)
            SCW = ar.alloc([128, 64], F32)
            MSEL = ar.alloc([128, 64], F32)
            M8 = ar.alloc([128, 16], F32)
            SS = ar.alloc([128, 16], F32)
            OEV = [ar.alloc([128, 4, 129], F32) for _ in range(3)]
            CF = ar.alloc([128, 3, 4], F32)
            OTOK = ar.alloc([128, 4, 128], F32)
            GM = ar.alloc([128, 4, 16], F32)
            MB = ar.alloc([128, 4, 16], F32)
            MBB = ar.alloc([128, 4, 16], BF16)
            st_n = [0]
            oset_n = [0]

            def oset():
                k = oset_n[0] % 2
                oset_n[0] += 1
                return (PS[3 + 2 * k], PS[4 + 2 * k])

            def oreg(os_, h):
                bk = os_[h // 2]
                return bk, bk.ap[:, (h % 2) * 129:(h % 2) * 129 + 129]

            def evac_o(os_, dst):
                for k in range(2):
                    src = os_[k].ap[:, 0:258].rearrange("p (h d) -> p h d", h=2)
                    if k == 0:
                        op("act", lambda e, src=src: e.copy(out=dst.ap[:, 0:2, :], in_=src), reads=[os_[k].b, dst.b], writes=[dst.b])
                    else:
                        op("dve", lambda e, src=src: e.tensor_copy(out=dst.ap[:, 2:4, :], in_=src), reads=[os_[k].b, dst.b], writes=[dst.b])

            def attn_step(kq_pairs, mask_list, v_aps, v_b, os_, first, last):
                bk = PS[st_n[0] % 3]
                pt = PTB[st_n[0] % 3]
                st_n[0] += 1
                for (c0, ncol, kl, qr, rd), masks in zip(kq_pairs, mask_list):
                    outap = bk.ap[:, c0:c0 + ncol]
                    op("pe", lambda e, outap=outap, kl=kl, qr=qr: e.matmul(outap, lhsT=kl, rhs=qr, start=True, stop=False),
                       reads=rd, writes=[bk.b])
                    for mi, (ml, mr, mrd) in enumerate(masks):
                        op("pe", lambda e, outap=outap, ml=ml, mr=mr, lastm=(mi == len(masks) - 1): e.matmul(
                            outap, lhsT=ml, rhs=mr, start=False, stop=lastm), reads=mrd, writes=[bk.b])
                op("act", lambda e, bk=bk, pt=pt: e.activation(out=pt.ap, in_=bk.ap, func=AF.Exp), reads=[bk.b], writes=[pt.b])
                if first:
                    for k in range(2):
                        op("pe", lambda e, k=k: e.matmul(os_[k].ap[:, 0:258], lhsT=ZR.ap[:, 0:128], rhs=ZR.ap, start=True, stop=False),
                           reads=[ZR.b], writes=[os_[k].b])
                for h in range(4):
                    obk, oap = oreg(os_, h)
                    op("pe", lambda e, oap=oap, pt=pt, h=h: e.matmul(oap, lhsT=pt.ap[:, h * 128:(h + 1) * 128], rhs=v_aps[h],
                                                                     start=False, stop=last), reads=[pt.b, v_b], writes=[obk.b])

            def finalize(osrcs, coefs_fn, feat0, i):
                nb = len(osrcs)
                for bi, oe in enumerate(osrcs):
                    op("dve", lambda e, oe=oe, bi=bi: e.tensor_scalar(out=CF.ap[:, bi, :], in0=oe.ap[:, :, 128], scalar1=1e-30, scalar2=None, op0=ALU.max),
                       reads=[oe.b, CF.b], writes=[CF.b])
                    op("dve", lambda e, bi=bi: e.reciprocal(out=CF.ap[:, bi, :], in_=CF.ap[:, bi, :]), reads=[CF.b], writes=[CF.b])
                    gap = coefs_fn(bi)
                    if gap is not None:
                        op("dve", lambda e, bi=bi, gap=gap: e.tensor_tensor(out=CF.ap[:, bi, :], in0=CF.ap[:, bi, :], in1=gap, op=ALU.mult),
                           reads=[CF.b, G.b], writes=[CF.b])
                for h in range(4):
                    op("dve", lambda e, h=h: e.tensor_scalar(out=OTOK.ap[:, h, :], in0=osrcs[0].ap[:, h, 0:128], scalar1=CF.ap[:, 0, h:h + 1],
                                                            scalar2=None, op0=ALU.mult), reads=[osrcs[0].b, CF.b, OTOK.b], writes=[OTOK.b])
                    for bi in range(1, nb):
                        op("dve", lambda e, h=h, bi=bi: e.scalar_tensor_tensor(
                            out=OTOK.ap[:, h, :], in0=osrcs[bi].ap[:, h, 0:128], scalar=CF.ap[:, bi, h:h + 1], in1=OTOK.ap[:, h, :],
                            op0=ALU.mult, op1=ALU.add), reads=[osrcs[bi].b, CF.b, OTOK.b], writes=[OTOK.b])
                bk = PS[7]
                for h in range(4):
                    op("pe", lambda e, h=h, bk=bk: e.transpose(out=bk.ap[:, h * 128:(h + 1) * 128], in_=OTOK.ap[:, h, :], identity=IDF.ap),
                       reads=[OTOK.b, IDF.b], writes=[bk.b])
                op("act", lambda e, bk=bk: e.copy(out=OT.ap[:, feat0:feat0 + 4, i * 128:(i + 1) * 128],
                                                  in_=bk.ap.rearrange("p (h q) -> p h q", h=4)), reads=[bk.b, OT.b], writes=[OT.b])

            for k in range(4):
                dma("sp", "ka%d" % k, KA[k].ap, KT[4 + k], reads=[ktb(4 + k, tc) for tc in range(8)], writes=[KA[k].b])
            for q4 in range(4):
                dma("sp", "va%d" % q4, VA[q4].ap,
                    VD[8 * q4:8 * q4 + 8, :, 0:516].rearrange("t p c -> p t c"),
                    reads=[vdb(gt, grp) for gt in range(8 * q4, 8 * q4 + 8) for grp in range(2)], writes=[VA[q4].b])
            for i in range(NL):
                nk = 4 * i + 4
                cmpf = CMPF[i % 2]
                dma("sp", "cf%d" % (i % 2), cmpf.ap, cmpf_d[:, i * 256:(i + 1) * 256], writes=[cmpf.b])
                for g in range(2):
                    qr4 = QT.ap[:, 4 * g:4 * g + 4, i * 128:(i + 1) * 128]
                    os_ = oset()
                    for h in range(4):
                        bk = os_[h // 2]
                        op("pe", lambda e, bk=bk, h=h, g=g, i=i: e.matmul(
                            bk.ap[:, (h % 2) * 256:(h % 2) * 256 + 256], lhsT=QT.ap[:, 4 * g + h, i * 128:(i + 1) * 128],
                            rhs=KCT.ap[:, g, :], start=True, stop=True), reads=[QT.b, KCT.b], writes=[bk.b])
                    for k in range(2):
                        op("dve", lambda e, k=k, os_=os_, cmpf=cmpf: e.tensor_tensor(
                            out=SM.ap[:, 2 * k:2 * k + 2, :], in0=os_[k].ap.rearrange("p (h c) -> p h c", h=2),
                            in1=cmpf.ap.unsqueeze(1).to_broadcast([128, 2, 256]), op=ALU.add),
                            reads=[os_[k].b, cmpf.b, SM.b], writes=[SM.b])
                    for h in range(4):
                        op("act", lambda e, h=h: e.activation(out=SM.ap[:, h, :], in_=SM.ap[:, h, :], func=AF.Exp, accum_out=SS.ap[:, h:h + 1]),
                           reads=[SM.b, SS.b], writes=[SM.b, SS.b])
                    op("dve", lambda e: e.tensor_scalar(out=SS.ap[:, 4:8], in0=SS.ap[:, 0:4], scalar1=1e-30, scalar2=None, op0=ALU.max),
                       reads=[SS.b], writes=[SS.b])
                    op("dve", lambda e: e.reciprocal(out=SS.ap[:, 8:12], in_=SS.ap[:, 4:8]), reads=[SS.b], writes=[SS.b])
                    op("dve", lambda e: e.tensor_scalar(out=PACC.ap, in0=SM.ap[:, 0, :], scalar1=SS.ap[:, 8:9], scalar2=None, op0=ALU.mult),
                       reads=[SM.b, SS.b], writes=[PACC.b])
                    for h in range(1, 4):
                        op("dve", lambda e, h=h: e.scalar_tensor_tensor(out=PACC.ap, in0=SM.ap[:, h, :], scalar=SS.ap[:, 8 + h:9 + h], in1=PACC.ap,
                                                                        op0=ALU.mult, op1=ALU.add), reads=[SM.b, SS.b, PACC.b], writes=[PACC.b])
                    P3 = PACC.ap.rearrange("p (s r) -> p s r", r=4)
                    op("dve", lambda e, P3=P3: e.tensor_tensor(out=IMP.ap, in0=P3[:, :, 0], in1=P3[:, :, 1], op=ALU.add), reads=[PACC.b], writes=[IMP.b])
                    op("dve", lambda e, P3=P3: e.tensor_tensor(out=IMP.ap, in0=IMP.ap, in1=P3[:, :, 2], op=ALU.add), reads=[PACC.b, IMP.b], writes=[IMP.b])
                    op("dve", lambda e, P3=P3: e.scalar_tensor_tensor(out=IMP.ap, in0=P3[:, :, 3], scalar=0.5, in1=IMP.ap, op0=ALU.mult, op1=ALU.add),
                       reads=[PACC.b, IMP.b], writes=[IMP.b])
                    op("dve", lambda e, P3=P3: e.scalar_tensor_tensor(out=IMP.ap[:, 1:64], in0=P3[:, 0:63, 3], scalar=0.5, in1=IMP.ap[:, 1:64],
                                                                      op0=ALU.mult, op1=ALU.add), reads=[PACC.b, IMP.b], writes=[IMP.b])
                    op("dve", lambda e, i=i: e.tensor_tensor(out=IMP.ap, in0=IMP.ap, in1=NSAB.ap[:, i, :], op=ALU.add),
                       reads=[IMP.b, NSAB.b], writes=[IMP.b])
                    op("dve", lambda e: e.max(out=M8.ap[:, 0:8], in_=IMP.ap), reads=[IMP.b, M8.b], writes=[M8.b])
                    op("dve", lambda e: e.match_replace(out=SCW.ap, in_to_replace=M8.ap[:, 0:8], in_values=IMP.ap, imm_value=-30000.0),
                       reads=[IMP.b, M8.b], writes=[SCW.b])
                    op("dve", lambda e: e.max(out=M8.ap[:, 8:16], in_=SCW.ap), reads=[SCW.b, M8.b], writes=[M8.b])
                    op("dve", lambda e: e.tensor_scalar(out=MSEL.ap, in0=IMP.ap, scalar1=M8.ap[:, 15:16], scalar2=1.0, op0=ALU.is_ge, op1=ALU.subtract),
                       reads=[IMP.b, M8.b], writes=[MSEL.b])
                    op("dve", lambda e, nk=nk: e.tensor_copy(
                        out=MEXP.ap[:, 0:nk * 128].rearrange("p (s k) -> p s k", k=64),
                        in_=MSEL.ap[:, 0:2 * nk].unsqueeze(2).to_broadcast([128, 2 * nk, 64])), reads=[MSEL.b], writes=[MEXP.b])
                    os_ = oset()
                    for ct in range(2):
                        attn_step([(0, 512, KCT.ap[:, g, ct * 128:(ct + 1) * 128], qr4, [KCT.b, QT.b])],
                                  [[(CMPB.ap[:, i, ct * 128:(ct + 1) * 128], IDB4.ap, [CMPB.b, IDB4.b])]],
                                  [VCP.ap[:, g, ct, :]] * 4, VCP.b, os_, ct == 0, ct == 1)
                    evac_o(os_, OEV[0])
                    os_ = oset()
                    k0 = max(0, 4 * i - 4)
                    for kt in range(k0, nk):
                        masks = [(WM.ap[:, kt - (4 * i - 4), :], IDB4.ap, [WM.b, IDB4.b])]
                        attn_step([(0, 512, KA[2 + g].ap[:, kt * 128:(kt + 1) * 128], qr4, [KA[2 + g].b, QT.b])], [masks],
                                  [VA[kt // 8].ap[:, kt % 8, (2 + g) * 129:(3 + g) * 129]] * 4, VA[kt // 8].b, os_, kt == k0, kt == nk - 1)
                    evac_o(os_, OEV[2])
                    os_ = oset()
                    for kt in range(nk):
                        masks = [(MEXP.ap[:, kt * 128:(kt + 1) * 128], IDB4.ap, [MEXP.b, IDB4.b])]
                        if kt >= 4 * i:
                            masks.append((CM.ap[:, kt - 4 * i, :], IDB4.ap, [CM.b, IDB4.b]))
                        attn_step([(0, 512, KA[g].ap[:, kt * 128:(kt + 1) * 128], qr4, [KA[g].b, QT.b])], [masks],
                                  [VA[kt // 8].ap[:, kt % 8, g * 129:(g + 1) * 129]] * 4, VA[kt // 8].b, os_, kt == 0, kt == nk - 1)
                    evac_o(os_, OEV[1])
                    G3 = G.ap[:, i, :].rearrange("p (h b) -> p h b", b=3)
                    finalize(OEV, lambda bi, G3=G3, g=g: G3[:, 4 * g:4 * g + 4, bi], 4 * g, i)
            for r in range(2):
                for k in range(4):
                    dma("sp", "ka%d" % k, KA[k].ap, KT[8 + 4 * r + k], reads=[ktb(8 + 4 * r + k, tc) for tc in range(8)], writes=[KA[k].b])
                for q4 in range(4):
                    dma("sp", "va%d" % q4, VA[q4].ap,
                        VD[8 * q4:8 * q4 + 8, :, (4 + 4 * r) * 129:(8 + 4 * r) * 129].rearrange("t p c -> p t c"),
                        reads=[vdb(gt, grp) for gt in range(8 * q4, 8 * q4 + 8) for grp in range(2 + 2 * r, 4 + 2 * r)], writes=[VA[q4].b])
                for i in range(NL):
                    nk = 4 * i + 4
                    bk7 = PS[7]
                    for h in range(4):
                        op("pe", lambda e, h=h, r=r, i=i: e.matmul(bk7.ap[:, h * 16:(h + 1) * 16], lhsT=QT.ap[:, 8 + 4 * r + h, i * 128:(i + 1) * 128],
                                                                   rhs=KMT.ap[:, 4 * r + h, :], start=True, stop=True), reads=[QT.b, KMT.b], writes=[bk7.b])
                    op("dve", lambda e, i=i: e.tensor_tensor(out=GM.ap, in0=bk7.ap[:, 0:64].rearrange("p (h n) -> p h n", h=4),
                                                             in1=MOBAB.ap[:, i:i + 1, :].to_broadcast([128, 4, 16]), op=ALU.add),
                       reads=[bk7.b, MOBAB.b], writes=[GM.b])
                    for h in range(4):
                        op("dve", lambda e, h=h: e.max(out=M8.ap[:, 0:8], in_=GM.ap[:, h, :]), reads=[GM.b, M8.b], writes=[M8.b])
                        op("dve", lambda e, h=h: e.tensor_scalar(out=MB.ap[:, h, :], in0=GM.ap[:, h, :], scalar1=M8.ap[:, 2:3], scalar2=1.0,
                                                                op0=ALU.is_ge, op1=ALU.subtract), reads=[GM.b, M8.b, MB.b], writes=[MB.b])
                    op("dve", lambda e, i=i: e.tensor_tensor(out=MBB.ap, in0=MB.ap, in1=MOBAV.ap[:, i:i + 1, :].to_broadcast([128, 4, 16]), op=ALU.mult),
                       reads=[MB.b, MOBAV.b], writes=[MBB.b])
                    os_ = oset()
                    for kt in range(nk):
                        pairs = []
                        masks_all = []
                        for h in range(4):
                            pairs.append((h * 128, 128, KA[h].ap[:, kt * 128:(kt + 1) * 128], QT.ap[:, 8 + 4 * r + h, i * 128:(i + 1) * 128], [KA[h].b, QT.b]))
                            ms = [(MBB.ap[:, h, kt // 2:kt // 2 + 1].to_broadcast([128, 128]), IDB4.ap[:, 0:128], [MBB.b, IDB4.b])]
                            if kt >= 4 * i:
                                ms.append((CM.ap[:, kt - 4 * i, :], IDB4.ap[:, 0:128], [CM.b, IDB4.b]))
                            masks_all.append(ms)
                        attn_step(pairs, masks_all, [VA[kt // 8].ap[:, kt % 8, h * 129:(h + 1) * 129] for h in range(4)], VA[kt // 8].b, os_, kt == 0, kt == nk - 1)
                    evac_o(os_, OEV[0])
                    finalize([OEV[0]], lambda bi: None, 8 + 4 * r, i)
            if debug:
                dma("sp", "dbg6", dbg_ot, OT.ap.rearrange("p h t -> p (h t)"), reads=[OT.b], writes=[dbg_b[6]])

        def bcast_vec(src_ap, src_b, dst):
            for c4 in range(4):
                bk = PS[c4]
                for cc in range(4):
                    c = 4 * c4 + cc
                    op("pe", lambda e, bk=bk, cc=cc, c=c: e.matmul(bk.ap[:, cc * 128:(cc + 1) * 128], lhsT=src_ap[:, c:c + 1].to_broadcast([128, 128]),
                                                                   rhs=IDF.ap, start=True, stop=True), reads=[src_b, IDF.b], writes=[bk.b])
                op("dve", lambda e, bk=bk, c4=c4: e.tensor_copy(out=dst.ap[:, c4 * 512:(c4 + 1) * 512], in_=bk.ap), reads=[bk.b, dst.b], writes=[dst.b])

        def rstd_from_ss(ss_ap, dst_ap, b):
            op("act", lambda e: e.activation(out=dst_ap, in_=ss_ap, func=AF.Sqrt, scale=1.0 / D, bias=EPS), reads=[b], writes=[b])
            op("dve", lambda e: e.reciprocal(out=dst_ap, in_=dst_ap), reads=[b], writes=[b])

        def phase_o(OT, H2):
            WO = ar.alloc([128, KC, D], BF16)
            GGM = ar.alloc([128, D], F32)
            XS = [ar.alloc([128, D], F32) for _ in range(2)]
            X1 = ar.alloc([128, D], F32)
            JN = ar.alloc([128, D], BF16)
            ST = [ar.alloc([128, 8], F32) for _ in range(2)]
            for cg in range(4):
                dma("pool", "wo%d" % cg, WO.ap[:, :, cg * 512:(cg + 1) * 512],
                    w_o_r[cg].rearrange("p (kc c) -> p kc c", kc=KC), writes=[WO.b])
            bcast_vec(GGS.ap[:, 0, :], GGS.b, GGM)
            for i in range(NL):
                bs = 4 * (i % 2)
                xs = XS[i % 2]
                x1 = X1
                stt = ST[i % 2]
                dma("sp", "xs%d" % (i % 2), xs.ap, xo[i * 128:(i + 1) * 128, :], writes=[xs.b])
                for cg in range(4):
                    bk = PS[bs + cg]
                    for kc in range(KC):
                        op("pe", lambda e, bk=bk, kc=kc, cg=cg, i=i: e.matmul(bk.ap, lhsT=OT.ap[:, kc, i * 128:(i + 1) * 128],
                                                                             rhs=WO.ap[:, kc, cg * 512:(cg + 1) * 512], start=(kc == 0), stop=(kc == KC - 1)),
                           reads=[OT.b, WO.b], writes=[bk.b])
                    op("act", lambda e, bk=bk, cg=cg, stt=stt: e.activation(out=JN.ap[:, 0:512], in_=bk.ap, func=AF.Square, accum_out=stt.ap[:, cg:cg + 1]),
                       reads=[bk.b, stt.b, JN.b], writes=[JN.b, stt.b])
                op("dve", lambda e, stt=stt: e.tensor_reduce(out=stt.ap[:, 4:5], in_=stt.ap[:, 0:4], axis=AX.X, op=ALU.add), reads=[stt.b], writes=[stt.b])
                rstd_from_ss(stt.ap[:, 4:5], stt.ap[:, 5:6], stt.b)
                for cg in range(4):
                    bk = PS[bs + cg]
                    op("dve", lambda e, bk=bk, cg=cg, stt=stt, x1=x1: e.scalar_tensor_tensor(
                        out=x1.ap[:, cg * 512:(cg + 1) * 512], in0=bk.ap, scalar=stt.ap[:, 5:6], in1=GGM.ap[:, cg * 512:(cg + 1) * 512],
                        op0=ALU.mult, op1=ALU.mult), reads=[bk.b, stt.b, GGM.b, x1.b], writes=[x1.b])
                op("dve", lambda e, x1=x1, xs=xs: e.tensor_tensor(out=x1.ap, in0=x1.ap, in1=xs.ap, op=ALU.add), reads=[x1.b, xs.b], writes=[x1.b])
                dma("sp", "x1s", X1S[i * 128:(i + 1) * 128, :], x1.ap, reads=[x1.b], writes=[x1_b[i]])
                norm_tile_to_hT(x1, JN, JN, H2, i * 128, AFF, MODT.ap[:, 48:64], (i + 1) % 2, i)

        def phase_f(H2):
            UT = ar.alloc([128, 64, NL * 128], BF16)
            WU = [ar.alloc([128, KC, 128], BF16) for _ in range(2)]
            RR = ar.alloc([128, 512], F32)
            n_ev = 0
            for fc in range(64):
                wu = WU[fc % 2]
                dma("pool", "wu%d" % (fc % 2), wu.ap, w_up_r[fc].rearrange("p (kc c) -> p kc c", kc=KC), writes=[wu.b])
                for half in range(2):
                    bk = PS[n_ev % 8]
                    for kc in range(KC):
                        op("pe", lambda e, bk=bk, kc=kc, wu=wu, half=half: e.matmul(bk.ap, lhsT=wu.ap[:, kc, :], rhs=H2.ap[:, kc, half * 512:(half + 1) * 512],
                                                                                  start=(kc == 0), stop=(kc == KC - 1)), reads=[wu.b, H2.b], writes=[bk.b])
                    op("dve", lambda e, bk=bk: e.tensor_scalar(out=RR.ap, in0=bk.ap, scalar1=0.0, scalar2=None, op0=ALU.max), reads=[bk.b, RR.b], writes=[RR.b])
                    op("dve", lambda e, fc=fc, half=half: e.tensor_tensor(out=UT.ap[:, fc, half * 512:(half + 1) * 512], in0=RR.ap, in1=RR.ap, op=ALU.mult),
                       reads=[RR.b, UT.b], writes=[UT.b])
                    n_ev += 1
            p.barrier()
            if stop_after == "f_up":
                return
            ar.top = SLOT1
            WD = [ar.alloc([128, 8, 512], BF16) for _ in range(2)]
            FSB = [ar.alloc([128, 512], F32) for _ in range(2)]
            JN = ar.alloc([128, 512], BF16)
            assert ar.top <= SLOT2
            n_w = 0
            n_f = 0
            for cg in range(4):
                for kg in range(8):
                    wd = WD[n_w % 2]
                    dma("pool", "wd%d" % (n_w % 2), wd.ap,
                        w_down_r[cg * 8 + kg].rearrange("p (kk c) -> p kk c", kk=8), writes=[wd.b])
                    n_w += 1
                    for kk in range(8):
                        kc = kg * 8 + kk
                        for i in range(NL):
                            bk = PS[i]
                            op("pe", lambda e, bk=bk, kc=kc, kk=kk, wd=wd, i=i: e.matmul(bk.ap, lhsT=UT.ap[:, kc, i * 128:(i + 1) * 128], rhs=wd.ap[:, kk, :],
                                                                                       start=(kc == 0), stop=(kc == 63)), reads=[UT.b, wd.b], writes=[bk.b])
                for i in range(NL):
                    bk = PS[i]
                    fsb_ = FSB[n_f % 2]
                    op("act", lambda e, bk=bk, i=i, cg=cg: e.activation(out=JN.ap, in_=bk.ap, func=AF.Square, accum_out=SSF.ap[:, i, cg:cg + 1]),
                       reads=[bk.b, SSF.b, JN.b], writes=[JN.b, SSF.b])
                    op("dve", lambda e, bk=bk, fsb_=fsb_: e.tensor_copy(out=fsb_.ap, in_=bk.ap), reads=[bk.b, fsb_.b, SSF.b], writes=[fsb_.b])
                    dma("sp", "fs%d" % (n_f % 2), FS[i * 128:(i + 1) * 128, cg * 512:(cg + 1) * 512], fsb_.ap, reads=[fsb_.b], writes=[fsb(i, cg)])
                    n_f += 1
            p.barrier()
            if stop_after == "f_down":
                return
            ar.top = SLOT1
            GGF = ar.alloc([128, D], F32)
            FT = [ar.alloc([128, D], F32) for _ in range(2)]
            XT = [ar.alloc([128, D], F32) for _ in range(2)]
            ST = ar.alloc([128, NL, 2], F32)
            bcast_vec(GGS.ap[:, 1, :], GGS.b, GGF)
            for i in range(NL):
                ft = FT[i % 2]
                xt = XT[i % 2]
                dma("sp", "ft%d" % (i % 2), ft.ap, FS[i * 128:(i + 1) * 128, :], reads=[fsb(i, cg) for cg in range(4)], writes=[ft.b])
                dma("sp", "xt%d" % (i % 2), xt.ap, X1S[i * 128:(i + 1) * 128, :], reads=[x1_b[i]], writes=[xt.b])
                op("dve", lambda e, i=i: e.tensor_reduce(out=ST.ap[:, i, 0:1], in_=SSF.ap[:, i, :], axis=AX.X, op=ALU.add), reads=[SSF.b, ST.b], writes=[ST.b])
                rstd_from_ss(ST.ap[:, i, 0:1], ST.ap[:, i, 1:2], ST.b)
                op("dve", lambda e, i=i, ft=ft: e.scalar_tensor_tensor(out=ft.ap, in0=ft.ap, scalar=ST.ap[:, i, 1:2], in1=GGF.ap, op0=ALU.mult, op1=ALU.mult),
                   reads=[ft.b, ST.b, GGF.b], writes=[ft.b])
                op("dve", lambda e, ft=ft, xt=xt: e.tensor_tensor(out=ft.ap, in0=ft.ap, in1=xt.ap, op=ALU.add), reads=[ft.b, xt.b], writes=[ft.b])
                dma("sp", "out%d" % (i % 2), out[i * 128:(i + 1) * 128, :], ft.ap, reads=[ft.b], writes=[out_b[i]])

        def run_all():
            phase_mod()
            p.barrier()
            if stop_after == "mod":
                return
            ar.top = persist_mark
            if "kv" not in skip:
                phase_kv()
            p.barrier()
            if stop_after == "kv":
                return
            ar.top = SLOT1
            QT = ar.alloc([128, 16, NL * 128], BF16)
            assert ar.top == SLOT2
            ar.top = SLOT3
            phase_q(QT)
            p.barrier()
            if stop_after == "q":
                return
            ar.top = SLOT3
            phase_c()
            p.barrier()
            if stop_after == "c":
                return
            ar.top = SLOT2
            OT = ar.alloc([128, 16, NL * 128], BF16)
            assert ar.top == PB
            ar.top = SLOT3
            if "a" not in skip:
                phase_a(QT, OT)
            p.barrier()
            if stop_after == "a":
                return
            ar.top = SLOT1
            H2 = ar.alloc([128, KC, NL * 128], BF16)
            ar.top = PB
            phase_o(OT, H2)
            p.barrier()
            if stop_after == "o":
                return
            ar.top = SLOT2
            phase_f(H2)

        run_all()
        fin = list(out_b)
        if debug:
            fin += dbg_b + list(kt_b.values()) + list(vd_b.values()) + x1_b + list(fs_b.values())
        p.final_wait("sp", fin)
        p.emit()
    return nc


def _tables():
    inv = (10000.0 ** (-np.arange(0, 128, 2, dtype=np.float32) / np.float32(128))).astype(np.float32)
    pos = np.arange(T, dtype=np.float32)
    ang = (pos[:, None] * inv[None, :]).astype(np.float32)
    cos = np.cos(ang).astype(np.float32).T
    sin = np.sin(ang).astype(np.float32).T
    cosk = np.concatenate([cos, cos], 0)
    sink = np.concatenate([sin, -sin], 0)
    return np.ascontiguousarray(cosk), np.ascontiguousarray(sink)


def _core_tables(j, cosk, sink):
    bf = ml_dtypes.bfloat16
    own = np.concatenate([np.arange(128 * (4 * i + j), 128 * (4 * i + j) + 128) for i in range(NL)])
    sc = np.float32(128 ** -0.5)
    cosq = np.ascontiguousarray(cosk[:, own] * sc).astype(np.float32)
    sinq = np.ascontiguousarray(sink[:, own] * sc).astype(np.float32)
    q = np.arange(128)[:, None]
    k = np.arange(128)[None, :]
    cm = np.zeros((128, 4, 128), np.float32)
    for jp in range(4):
        if jp == j:
            cm[:, jp, :] = np.where(k <= q, 0.0, -1.0)
        elif jp > j:
            cm[:, jp, :] = -1.0
    wm = np.zeros((128, 8, 128), np.float32)
    for jj in range(8):
        dist = 128 * (j + 4 - jj) + q - k
        wm[:, jj, :] = np.where((dist >= 0) & (dist < 512), 0.0, -1.0)
    cmpf = np.zeros((128, NL, 256), np.float32)
    nsab = np.zeros((128, NL, 64), np.float32)
    mobab = np.zeros((128, NL, 16), np.float32)
    mobav = np.zeros((128, NL, 16), np.float32)
    c = np.arange(256)[None, :]
    s = np.arange(64)[None, :]
    n = np.arange(16)[None, :]
    for i in range(NL):
        tq = 128 * (4 * i + j) + np.arange(128)[:, None]
        vis = (16 * c + 31 <= tq) & (c < 255)
        cmpf[:, i, :] = np.where(vis, 0.0, -30000.0)
        cb = tq // 64
        forced = (s == 0) | (s == cb) | (s == cb - 1)
        nsab[:, i, :] = np.where(s > cb, -10000.0, np.where(forced, 10000.0, 0.0))
        cur = (4 * i + j) // 2
        mobab[:, i, :] = np.where(n < cur, 0.0, -30000.0)
        mobav[:, i, :] = np.where(n < cur, 1.0, 0.0)
    cmpb = np.where(cmpf < 0, -1.0, 0.0)
    return dict(
        cosq=cosq, sinq=sinq,
        cmask=cm.reshape(128, -1).astype(bf), wmask=wm.reshape(128, -1).astype(bf),
        cmpf=cmpf.reshape(128, -1), cmpb=cmpb.reshape(128, -1).astype(bf),
        nsab=nsab.reshape(128, -1), mobab=mobab.reshape(128, -1), mobav=mobav.reshape(128, -1),
    )


def make_in_maps(inputs):
    bf = ml_dtypes.bfloat16
    f = lambda a: np.ascontiguousarray(np.asarray(a), dtype=np.float32)
    x = f(inputs["x"])
    c = f(inputs["c"])
    wadaT = np.ascontiguousarray(f(inputs["w_ada"])[0].T)
    bada = np.ascontiguousarray(f(inputs["b_ada"])[0].reshape(96, 128).T)
    norms = np.stack([f(inputs["pre_norm_mix"])[0], f(inputs["post_norm_mix"])[0],
                      f(inputs["pre_norm_ffn"])[0], f(inputs["post_norm_ffn"])[0]], 0)
    normsT = np.ascontiguousarray(norms.reshape(4, 16, 128).transpose(2, 0, 1).reshape(128, 64))
    cosk, sink = _tables()
    w_in = f(inputs["w_in"])[0]
    grp_cols = [FM[2 * g][0] for g in range(8)] + [TM[2 * g] for g in range(6)] + \
               [C_QN + 256 * g for g in range(4)] + [C_QM + 256 * g for g in range(4)]

    def pm(w, ncol):
        return np.ascontiguousarray(w.reshape(-1, 128, ncol).transpose(1, 0, 2).reshape(128, -1))
    w_in_r = np.stack([pm(w_in[:, c0:c0 + 256], 256) for c0 in grp_cols], 0)
    w_g_r = pm(w_in[:, C_G:C_G + 24], 24)
    w_o = f(inputs["w_o"])[0]
    w_o_r = np.stack([pm(w_o[:, cg * 512:(cg + 1) * 512], 512) for cg in range(4)], 0)
    w_up = f(inputs["w_up"])[0]
    w_up_r = np.stack([pm(w_up[:, fc * 128:(fc + 1) * 128], 128) for fc in range(64)], 0)
    w_down = f(inputs["w_down"])[0]
    w_down_r = np.stack([pm(w_down[kg * 1024:(kg + 1) * 1024, cg * 512:(cg + 1) * 512], 512) for cg in range(4) for kg in range(8)], 0)

    def pm1(w):
        return np.ascontiguousarray(w.reshape(32, 128, 128).transpose(1, 0, 2).reshape(128, -1))
    shared = dict(
        wadaT=wadaT, bada=bada, normsT=normsT, w_in_r=w_in_r, w_g_r=w_g_r,
        ckw1=pm1(f(inputs["cmp_k_w1"])[0]), cvw1=pm1(f(inputs["cmp_v_w1"])[0]),
        ckw2=f(inputs["cmp_k_w2"])[0], cvw2=f(inputs["cmp_v_w2"])[0],
        ckposT=np.ascontiguousarray(f(inputs["cmp_k_pos"])[0].T), cvposT=np.ascontiguousarray(f(inputs["cmp_v_pos"])[0].T),
        w_o_r=w_o_r, w_up_r=w_up_r, w_down_r=w_down_r,
        cosk=cosk, sink=sink,
        idb4=(np.tile(np.eye(128, dtype=np.float32), (1, 4)) * NEG).astype(bf),
        idf=np.eye(128, dtype=np.float32),
    )
    ctabs = [_core_tables(j, cosk, sink) for j in range(4)]
    in_maps = []
    for core in range(8):
        b, j = core // 4, core % 4
        m = dict(shared)
        m.update(ctabs[j])
        m["xb"] = x[b]
        m["xo"] = np.ascontiguousarray(x[b].reshape(NL, 4, 128, D)[:, j].reshape(NL * 128, D))
        m["cbc"] = np.ascontiguousarray(np.broadcast_to(c[b][None, :], (128, D)))
        in_maps.append(m)
    return in_maps


_NC_CACHE = {}


def kernel(**inputs):
    if "nc" not in _NC_CACHE:
        _NC_CACHE["nc"] = build_program(debug=False)
    nc = _NC_CACHE["nc"]
    in_maps = make_in_maps(inputs)
    res = run_bass_kernel_spmd(nc, in_maps, core_ids=list(range(8)))
    outp = np.zeros((2, T, D), np.float32)
    for core in range(8):
        b, j = core // 4, core % 4
        outp[b].reshape(NL, 4, 128, D)[:, j] = np.asarray(res.results[core]["out"]).reshape(NL, 128, D)
    return outp
```
